# Optimizing a Trainium2 kernel written in Bass

```python
import math
import jax
import jax.numpy as jnp
from jax import lax
import numpy as np

D_MODEL = 1024
BATCH = 16
SEQ = 4096
DEPTH = 2

N_MIXERS = 2
N_GLA_LAYERS = (DEPTH + 1) // 2
N_HGRN_LAYERS = DEPTH // 2
CHUNK = 64
NORM_EPS = 1e-6

GLA_HEADS = 4
GLA_KEY = D_MODEL // 2
GLA_VAL = D_MODEL
GLA_DK = GLA_KEY // GLA_HEADS
GLA_DV = GLA_VAL // GLA_HEADS
GLA_GATE_RANK = 16
GLA_GATE_NORMALIZER = 16.0
GLA_IN = 2 * GLA_KEY + 2 * GLA_VAL + GLA_GATE_RANK

HG_EXPAND = 128
HG_HEADS = D_MODEL // HG_EXPAND
HG_DK = HG_EXPAND
HG_DV = D_MODEL // HG_HEADS
HG_KEY = HG_HEADS * HG_DK
HG_VAL = HG_HEADS * HG_DV
HG_IN = 2 * HG_KEY + 2 * HG_VAL

D_FF = int(math.ceil(8 * D_MODEL / 3 / 256)) * 256

kernel_name = "gla_hgrn2_interleaved_hybrid"


def rms_norm(x, gain):
    xf = x.astype(jnp.float32)
    inv = lax.rsqrt(jnp.mean(xf * xf, axis=-1, keepdims=True) + NORM_EPS)
    return (xf * inv).astype(x.dtype) * gain


def chunked_gated_linear_attention(q, k, v, g_log):
    out_dtype = v.dtype
    B, S, H, DK = q.shape
    DV = v.shape[-1]
    N = S // CHUNK

    def to_chunks(t):
        return t.astype(jnp.float32).reshape(B, N, CHUNK, H, t.shape[-1]).transpose(1, 0, 3, 2, 4)

    qc, kc, vc, gc = to_chunks(q), to_chunks(k), to_chunks(v), to_chunks(g_log)
    causal = jnp.tril(jnp.ones((CHUNK, CHUNK), dtype=bool))[:, :, None]

    def step(state, inp):
        q_c, k_c, v_c, g_c = inp
        b = jnp.cumsum(g_c, axis=2)
        o_inter = jnp.einsum('bhcd,bhde->bhce', q_c * jnp.exp(b), state)
        diff = b[:, :, :, None, :] - b[:, :, None, :, :]
        decay = jnp.exp(jnp.where(causal, diff, -jnp.inf))
        scores = jnp.einsum('bhid,bhjd,bhijd->bhij', q_c, k_c, decay)
        o = o_inter + jnp.einsum('bhij,bhje->bhie', scores, v_c)
        b_last = b[:, :, -1:, :]
        k_dec = k_c * jnp.exp(b_last - b)
        state = jnp.exp(b_last[:, :, 0, :])[..., None] * state + jnp.einsum('bhcd,bhce->bhde', k_dec, v_c)
        return state, o

    state0 = jnp.zeros((B, H, DK, DV), jnp.float32)
    _, o = lax.scan(step, state0, (qc, kc, vc, gc))
    return o.transpose(1, 0, 3, 2, 4).reshape(B, S, H, DV).astype(out_dtype)


def gla_mixer(h, w_in, w_gate_up, b_gate, head_norm, w_out):
    B, S, _ = h.shape
    proj = h @ w_in
    q, k, v, r, gd = jnp.split(
        proj, [GLA_KEY, 2 * GLA_KEY, 2 * GLA_KEY + GLA_VAL, 2 * GLA_KEY + 2 * GLA_VAL], axis=-1)
    g_log = jax.nn.log_sigmoid((gd @ w_gate_up + b_gate).astype(jnp.float32)) / GLA_GATE_NORMALIZER
    q = q.reshape(B, S, GLA_HEADS, GLA_DK) * (GLA_DK ** -0.5)
    k = k.reshape(B, S, GLA_HEADS, GLA_DK)
    v = v.reshape(B, S, GLA_HEADS, GLA_DV)
    g_log = g_log.reshape(B, S, GLA_HEADS, GLA_DK)
    o = chunked_gated_linear_attention(q, k, v, g_log)
    o = rms_norm(o, head_norm) * jax.nn.silu(r.reshape(B, S, GLA_HEADS, GLA_DV))
    return o.reshape(B, S, GLA_VAL) @ w_out


def hgrn2_mixer(h, w_in, lb, out_norm, w_out):
    B, S, _ = h.shape
    proj = h @ w_in
    q, f, i, g = jnp.split(proj, [HG_KEY, 2 * HG_KEY, 2 * HG_KEY + HG_VAL], axis=-1)
    q = jax.nn.silu(q)
    f = f.astype(jnp.float32)
    log_forget = jnp.logaddexp(jnp.log(lb), jnp.log1p(-lb) + jax.nn.log_sigmoid(f))
    k = (1.0 - lb) * jax.nn.sigmoid(-f)
    q = q.reshape(B, S, HG_HEADS, HG_DK) * (HG_DK ** -0.5)
    k = k.reshape(B, S, HG_HEADS, HG_DK)
    log_forget = log_forget.reshape(B, S, HG_HEADS, HG_DK)
    v = i.reshape(B, S, HG_HEADS, HG_DV)
    o = chunked_gated_linear_attention(q, k, v, log_forget).reshape(B, S, HG_VAL)
    o = rms_norm(o, out_norm) * jax.nn.silu(g)
    return o @ w_out


def swiglu_ffn(h, w_in, w_out):
    gate, up = jnp.split(h @ w_in, [D_FF], axis=-1)
    return (jax.nn.silu(gate) * up) @ w_out


def setup_inputs(seed: int = 0) -> dict:
    key = jax.random.key(seed)
    ks = jax.random.split(key, 20)
    f32 = jnp.float32

    def w(k, shape, fan_in):
        return jax.random.normal(k, shape, f32) * (fan_in ** -0.5)

    def gain(k, shape):
        return 1.0 + 0.02 * jax.random.normal(k, shape, f32)

    return {
        "x": jax.random.normal(ks[0], (BATCH, SEQ, D_MODEL), f32),
        "mixer_norm": gain(ks[1], (DEPTH, D_MODEL)),
        "ffn_norm": gain(ks[2], (DEPTH, D_MODEL)),
        "gla_w_in": w(ks[3], (N_GLA_LAYERS, D_MODEL, GLA_IN), D_MODEL),
        "gla_w_gate_up": w(ks[4], (N_GLA_LAYERS, GLA_GATE_RANK, GLA_KEY), GLA_GATE_RANK),
        "gla_b_gate": 0.1 * jax.random.normal(ks[5], (N_GLA_LAYERS, GLA_KEY), f32),
        "gla_head_norm": gain(ks[6], (N_GLA_LAYERS, GLA_DV)),
        "gla_w_out": w(ks[7], (N_GLA_LAYERS, GLA_VAL, D_MODEL), GLA_VAL),
        "hgrn_w_in": w(ks[8], (N_HGRN_LAYERS, D_MODEL, HG_IN), D_MODEL),
        "hgrn_lower_bounds": 0.5 * jax.random.normal(ks[9], (DEPTH, HG_KEY), f32),
        "hgrn_out_norm": gain(ks[10], (N_HGRN_LAYERS, HG_VAL)),
        "hgrn_w_out": w(ks[11], (N_HGRN_LAYERS, HG_VAL, D_MODEL), HG_VAL),
        "ffn_w_in": w(ks[12], (DEPTH, D_MODEL, 2 * D_FF), D_MODEL),
        "ffn_w_out": w(ks[13], (DEPTH, D_FF, D_MODEL), D_FF),
        "final_norm": gain(ks[14], (D_MODEL,)),
    }


def reference(x, mixer_norm, ffn_norm, gla_w_in, gla_w_gate_up, gla_b_gate, gla_head_norm,
              gla_w_out, hgrn_w_in, hgrn_lower_bounds, hgrn_out_norm, hgrn_w_out,
              ffn_w_in, ffn_w_out, final_norm):
    lb_soft = jax.nn.softmax(hgrn_lower_bounds.astype(jnp.float32), axis=0)
    lb_table = jnp.cumsum(lb_soft, axis=0) - lb_soft[0]

    for layer in range(DEPTH):
        h = rms_norm(x, mixer_norm[layer])
        j = layer // N_MIXERS
        if layer % N_MIXERS == 0:
            mixed = gla_mixer(h, gla_w_in[j], gla_w_gate_up[j], gla_b_gate[j],
                              gla_head_norm[j], gla_w_out[j])
        else:
            mixed = hgrn2_mixer(h, hgrn_w_in[j], lb_table[layer], hgrn_out_norm[j], hgrn_w_out[j])
        x = x + mixed
        x = x + swiglu_ffn(rms_norm(x, ffn_norm[layer]), ffn_w_in[layer], ffn_w_out[layer])
    return rms_norm(x, final_norm)
```

```python
import math
from contextlib import ExitStack

import numpy as np
import concourse.bass as bass
import concourse.mybir as mybir
from concourse.bass_utils import run_bass_kernel_spmd

F32 = mybir.dt.float32
BF16 = mybir.dt.bfloat16
AF = mybir.ActivationFunctionType
ALU = mybir.AluOpType

D = 1024
DFF = 2816
T = 512
NTT = 4
NCH = 8
EPS = 1e-6
NSLOT = 5
NCORES = 8


class Op:
    __slots__ = ("eng", "meth", "args", "kw", "deps", "dma_sem", "sem", "val", "inc", "waits")


class Sched:
    def __init__(self):
        self.ops = []
        self.last_writer = {}
        self.readers = {}
        self.last_dma = {}

    def add(self, eng, meth, *args, reads=(), writes=(), dma_sem=None, **kw):
        idx = len(self.ops)
        deps = []
        lw = self.last_writer
        for r in reads:
            w = lw.get(r)
            if w is not None:
                deps.append((w, 0))
        for r in writes:
            w = lw.get(r)
            if w is not None:
                deps.append((w, 1))
            for rd in self.readers.get(r, ()):
                deps.append((rd, 1))
        if dma_sem is not None:
            pd = self.last_dma.get(dma_sem)
            if pd is not None:
                deps.append((pd, 0))
            self.last_dma[dma_sem] = idx
        for r in reads:
            self.readers.setdefault(r, []).append(idx)
        for r in writes:
            lw[r] = idx
            self.readers[r] = []
        op = Op()
        op.eng, op.meth, op.args, op.kw = eng, meth, args, kw
        op.deps, op.dma_sem = deps, dma_sem
        self.ops.append(op)
        return idx

    @staticmethod
    def _skip(p, c, kind):
        if p.eng == c.eng and p.dma_sem is None and c.dma_sem is None:
            if p.eng == "pe":
                return True
        return False

    def resolve(self):
        ops = self.ops
        needed = [False] * len(ops)
        for op in ops:
            for d, kind in op.deps:
                if not self._skip(ops[d], op, kind):
                    needed[d] = True
        cnt = {}
        for i, op in enumerate(ops):
            if op.dma_sem is not None:
                op.sem = op.dma_sem
                cnt[op.sem] = cnt.get(op.sem, 0) + 16
                op.val, op.inc = cnt[op.sem], 16
            elif needed[i] and op.meth is not None:
                op.sem = "E_" + op.eng
                cnt[op.sem] = cnt.get(op.sem, 0) + 1
                op.val, op.inc = cnt[op.sem], 1
            else:
                op.sem, op.val, op.inc = None, 0, 0
        eng_clock = {}
        done = [None] * len(ops)
        for i, op in enumerate(ops):
            clk = eng_clock.setdefault(op.eng, {})
            waits = {}
            for d, kind in op.deps:
                p = ops[d]
                if self._skip(p, op, kind):
                    continue
                if clk.get(p.sem, 0) >= p.val:
                    continue
                if waits.get(p.sem, 0) < p.val:
                    waits[p.sem] = p.val
                for k, v in done[d].items():
                    if clk.get(k, 0) < v:
                        clk[k] = v
            op.waits = list(waits.items())
            if op.sem is not None:
                dc = dict(clk)
                dc[op.sem] = op.val
                done[i] = dc
        return cnt

    def emit(self, nc, es):
        cnt = self.resolve()
        sems = {}
        for name in sorted(cnt):
            sems[name] = es.enter_context(nc.semaphore(name))
        per_eng = {}
        for op in self.ops:
            per_eng.setdefault(op.eng, []).append(op)

        def run(engname, e):
            for op in per_eng.get(engname, ()):
                for s, v in op.waits:
                    e.wait_ge(sems[s], v)
                if op.meth is None:
                    continue
                ins = getattr(e, op.meth)(*op.args, **op.kw)
                if op.inc:
                    ins.then_inc(sems[op.sem], op.inc)

        block = es.enter_context(nc.Block())

        @block.tensor
        def _(e):
            run("pe", e)

        @block.scalar
        def _(e):
            run("act", e)

        @block.vector
        def _(e):
            run("dve", e)

        @block.gpsimd
        def _(e):
            run("pool", e)

        @block.sync
        def _(e):
            run("sp", e)


def weight_blocks():
    blks = []
    for c0 in (1024, 1536, 512, 2048, 2560, 0):
        blks.append(("gla_w_in", 0, 0, 8, [(c0, 512)]))
    for c0 in (0, 512):
        blks.append(("gla_w_out", 0, 0, 8, [(c0, 512)]))
    for layer in (0, 1):
        if layer == 1:
            for c0 in (1024, 1536, 2048, 2560, 0, 512, 3072, 3584):
                blks.append(("hgrn_w_in", 0, 0, 8, [(c0, 512)]))
            for c0 in (0, 512):
                blks.append(("hgrn_w_out", 0, 0, 8, [(c0, 512)]))
        for j in range(11):
            blks.append(("ffn_w_in", layer, 0, 8, [(256 * j, 256), (DFF + 256 * j, 256)]))
        for ob in range(2):
            for kr in range(3):
                nk = 8 if kr < 2 else 6
                blks.append(("ffn_w_out", layer, 8 * kr, nk, [(512 * ob, 512)]))
    return blks


WB = weight_blocks()
NBLK = len(WB)


def build_program(ntiles, tiles_per_seq, layers=(0, 1), final_norm=True):
    ntok = ntiles * T
    nc = bass.Bass("TRN2", target_bir_lowering=False)
    S = Sched()

    def din(name, shape):
        return nc.dram_tensor(name, list(shape), F32, kind="ExternalInput").ap()

    x_in = din("x", (ntok, D))
    dram = {
        "mixer_norm": din("mixer_norm", (2, D)),
        "ffn_norm": din("ffn_norm", (2, D)),
        "gla_w_in": din("gla_w_in", (1, D, 3088)),
        "gla_w_gate_up": din("gla_w_gate_up", (1, 16, 512)),
        "gla_b_gate": din("gla_b_gate", (1, 512)),
        "gla_head_norm": din("gla_head_norm", (1, 256)),
        "gla_w_out": din("gla_w_out", (1, D, D)),
        "hgrn_w_in": din("hgrn_w_in", (1, D, 4096)),
        "hgrn_lower_bounds": din("hgrn_lower_bounds", (2, D)),
        "hgrn_out_norm": din("hgrn_out_norm", (1, D)),
        "hgrn_w_out": din("hgrn_w_out", (1, D, D)),
        "ffn_w_in": din("ffn_w_in", (2, D, 2 * DFF)),
        "ffn_w_out": din("ffn_w_out", (2, DFF, D)),
        "final_norm": din("final_norm", (D,)),
    }
    y_out = nc.dram_tensor("y", [ntok, D], F32, kind="ExternalOutput").ap()
    wsc = nc.dram_tensor("wsc", [NBLK, 128, 4096], BF16, kind="Internal").ap()

    es = ExitStack()
    with es:
        def sb(name, cols, dt, parts=128):
            return es.enter_context(nc.sbuf_tensor(name, [parts, cols], dt))

        XB = [sb("XB0", 4096, F32), sb("XB1", 4096, F32)]
        hpre = sb("hpre", 2048, BF16)
        hT = sb("hT", 4096, BF16)
        wsbig = sb("wsbig", NSLOT * 4096, BF16)
        ws = [wsbig[:, i * 4096:(i + 1) * 4096] for i in range(NSLOT)]
        gL = sb("gL", 1024, F32)
        bcr = sb("bcr", 1536, F32)
        qb = sb("qb", 4096, BF16)
        kbog = sb("kbog", 4096, BF16)
        kd_tok = sb("kd_tok", 4096, BF16)
        v_tok = sb("v_tok", 4096, BF16)
        gate = sb("gate", 4096, BF16)
        S_fs = [sb(f"S_f{i}", 1024, F32) for i in range(2)]
        Sbf = [sb(f"Sbf{i}", 1024, BF16) for i in range(9)]
        act = sb("act", 22 * 512, BF16)
        osq = sb("osq", 1024, BF16)
        rstd_t = sb("rstd_t", 512, F32)
        tmpA = sb("tmpA", 1536, F32)
        sg = sb("sg", 1024, F32)
        onesf = tmpA[:, 0:128]
        wgu_f = sg[:, 0:512]
        wgd_f = sg[:, 512:640]
        maskc = sb("maskc", 512, F32)
        mask4 = sb("mask4", 512, BF16)
        ident = sb("ident", 128, BF16)
        ones_bf = sb("ones_bf", 128, BF16)
        gainT = sb("gainT", 40, F32)
        ognorm0 = sb("ognorm0", 2, F32)
        ognorm1 = sb("ognorm1", 8, F32)
        negbg = sb("negbg", 4, F32)
        hb = sb("hb", 16, F32)
        lb_t = sb("lb_t", 8, F32)
        oml = sb("oml", 8, F32)
        noml = sb("noml", 8, F32)
        gfin = sb("gfin", 1024, F32)
        wgd = sb("wgd", 128, BF16)
        wgu = sb("wgu", 512, BF16)
        gd_sb = sb("gd_sb", 512, BF16)
        ss = sb("ss", 4, F32)
        rs = sb("rs", 4, F32)
        rsd_tok = sb("rsd_tok", 4, F32)
        eblast = sb("eblast", 64, F32)
        cst = sb("cst", 4, F32)
        ps = [es.enter_context(nc.psum_tensor(f"ps{i}", [128, 512], F32)) for i in range(8)]

        t1 = act[:, 0:8192].bitcast(F32)
        kdT_all = act

        cur = {}

        def set_tile_bufs(t):
            p = t % 2
            cur["xp"], cur["ep"] = p, 1 - p
            cur["x"] = XB[p]
            cur["eqk"] = XB[1 - p]
            cur["AT"] = XB[1 - p][:, 0:2048].bitcast(BF16)

        def xres(tt, p=None):
            p = cur["xp"] if p is None else p
            return [("XB", p, 2 * tt), ("XB", p, 2 * tt + 1)]

        def xall(p=None):
            p = cur["xp"] if p is None else p
            return [("XB", p, g) for g in range(8)]

        def eres(g):
            return ("XB", cur["ep"], g)

        pstate = {"i": 0}
        pinned = set()

        def bank(pin=False):
            while True:
                k = pstate["i"] % 8
                pstate["i"] += 1
                if k not in pinned:
                    break
            if pin:
                pinned.add(k)
            return k

        csem = "D_const"

        def cload(out_ap, in_ap, res):
            S.add("pool", "dma_start", out=out_ap, in_=in_ap, allow_slow_non_contiguous=True,
                  writes=[res], dma_sem=csem)

        S.add("dve", "memset", cst[:, 0:1], EPS, writes=["cst"])
        S.add("dve", "memset", cst[:, 1:2], 1.0, writes=["cst"])
        S.add("dve", "memset", cst[:, 2:3], math.log(128 ** -0.5), writes=["cst"])
        S.add("dve", "memset", cst[:, 3:4], 0.0, writes=["cst"])
        S.add("dve", "memset", maskc[:, :], 1.0, writes=["maskc"])
        S.add("dve", "memset", maskc[:, 0:512:64], 0.0, writes=["maskc"])
        S.add("dve", "memset", onesf, 1.0, writes=[("tmpA", 0)])
        S.add("dve", "tensor_copy", out=ones_bf[:, :], in_=onesf, reads=[("tmpA", 0)], writes=["ones_bf"])
        S.add("pool", "affine_select", out=ident[:, :], in_=ones_bf[:, :], pattern=[[-1, 128]],
              compare_op=ALU.is_equal, fill=0.0, base=0, channel_multiplier=1,
              reads=["ones_bf"], writes=["ident"])
        S.add("pool", "affine_select", out=mask4[:, 0:128], in_=onesf, pattern=[[1, 128]],
              compare_op=ALU.is_ge, fill=0.0, base=0, channel_multiplier=-1,
              reads=[("tmpA", 0)], writes=["mask4"])
        S.add("pool", "memset", mask4[0:64, 64:128], 0.0, writes=["mask4"])
        for r in range(1, 4):
            S.add("pool", "tensor_copy", out=mask4[:, r * 128:(r + 1) * 128], in_=mask4[:, 0:128],
                  reads=["mask4"], writes=["mask4"])

        for li in range(2):
            cload(gainT[:, li * 8:(li + 1) * 8], dram["mixer_norm"][li, :].rearrange("(kc p) -> p kc", p=128), "gainT")
            cload(gainT[:, 16 + li * 8:16 + (li + 1) * 8], dram["ffn_norm"][li, :].rearrange("(kc p) -> p kc", p=128), "gainT")
        cload(ognorm0[:, :], dram["gla_head_norm"][0, :].rearrange("(kc p) -> p kc", p=128), "ognorm0")
        cload(ognorm1[:, :], dram["hgrn_out_norm"][0, :].rearrange("(kc p) -> p kc", p=128), "ognorm1")
        cload(negbg[:, :], dram["gla_b_gate"][0, :].rearrange("(kc p) -> p kc", p=128), "negbg")
        S.add("dve", "tensor_scalar", out=negbg[:, :], in0=negbg[:, :], scalar1=-1.0, scalar2=None, op0=ALU.mult,
              reads=["negbg"], writes=["negbg"])
        for li in range(2):
            cload(hb[:, li * 8:(li + 1) * 8], dram["hgrn_lower_bounds"][li, :].rearrange("(kc p) -> p kc", p=128), "hb")
        S.add("dve", "tensor_tensor", out=lb_t[:, :], in0=hb[:, 8:16], in1=hb[:, 0:8], op=ALU.subtract,
              reads=["hb"], writes=["lb"])
        S.add("act", "activation", out=lb_t[:, :], in_=lb_t[:, :], func=AF.Sigmoid, reads=["lb"], writes=["lb"])
        S.add("dve", "tensor_scalar", out=oml[:, :], in0=lb_t[:, :], scalar1=-1.0, scalar2=1.0, op0=ALU.mult, op1=ALU.add,
              reads=["lb"], writes=["oml"])
        S.add("dve", "tensor_scalar", out=noml[:, :], in0=lb_t[:, :], scalar1=-1.0, scalar2=None, op0=ALU.add,
              reads=["lb"], writes=["noml"])
        cload(gfin[:, :], dram["final_norm"].partition_broadcast(128), "gfin")
        cload(wgd_f.rearrange("p (kc f) -> p kc f", f=16),
              dram["gla_w_in"][0, :, 3072:3088].rearrange("(kc p) f -> p kc f", p=128), ("sg", 1))
        for kc in range(8):
            S.add("dve", "tensor_scalar", out=wgd[:, kc * 16:(kc + 1) * 16], in0=wgd_f[:, kc * 16:(kc + 1) * 16],
                  scalar1=gainT[:, kc:kc + 1], scalar2=None, op0=ALU.mult,
                  reads=[("sg", 1), "gainT"], writes=["wgd"])
        cload(wgu_f[0:16, :], dram["gla_w_gate_up"][0, :, :], ("sg", 0))
        S.add("dve", "tensor_copy", out=wgu[0:16, :], in_=wgu_f[0:16, :], reads=[("sg", 0)], writes=["wgu"])

        stage_f = [(XB[0][:, :], [("XB", 0, g) for g in range(8)]),
                   (XB[1][:, :], [("XB", 1, g) for g in range(8)]),
                   (act[:, 0:8192].bitcast(F32), [("act", c) for c in range(16)]),
                   (wsbig[:, 0:8192].bitcast(F32), [("ws", 0), ("ws", 1)]),
                   (wsbig[:, 8192:16384].bitcast(F32), [("ws", 2), ("ws", 3)])]
        stage_b = [(hT[:, :], [("hT", tt) for tt in range(4)]), (qb[:, :], [("qb", h) for h in range(8)]),
                   (kbog[:, :], [("kbog", h) for h in range(8)]), (kd_tok[:, :], [("kd_tok", h) for h in range(8)]),
                   (v_tok[:, :], [("v_tok", tt) for tt in range(4)])]
        NST = len(stage_f)
        used_blocks = set()
        for b in range(NBLK):
            lay = 0 if b < 25 else 1
            if lay in layers:
                used_blocks.add(b)
        ub = sorted(used_blocks)

        def pl_load(pi):
            src, li, k0, nk, cols = WB[ub[pi]]
            sf_ap, sf_res = stage_f[pi % NST]
            c0 = 0
            for (col, w) in cols:
                S.add("sp", "dma_start",
                      out=sf_ap[:, 0:nk * 512].rearrange("p (kc f) -> p kc f", f=512)[:, :, c0:c0 + w],
                      in_=dram[src][li, k0 * 128:(k0 + nk) * 128, col:col + w].rearrange("(kc p) f -> p kc f", p=128),
                      writes=sf_res, dma_sem=f"D_pl{pi % NST}")
                c0 += w

        def pl_cast_store(pi):
            b = ub[pi]
            nk = WB[b][3]
            sf_ap, sf_res = stage_f[pi % NST]
            sbb, sb_res = stage_b[pi % NST]
            src, li = WB[b][0], WB[b][1]
            gi = {"gla_w_in": 0, "hgrn_w_in": 1}.get(src, (2 + li) if src == "ffn_w_in" else None)
            if gi is not None:
                for kc in range(nk):
                    gsc = gainT[:, gi * 8 + kc:gi * 8 + kc + 1]
                    if pi % 2 == 0:
                        S.add("act", "activation", out=sbb[:, kc * 512:(kc + 1) * 512], in_=sf_ap[:, kc * 512:(kc + 1) * 512],
                              func=AF.Copy, scale=gsc, reads=sf_res + ["gainT"], writes=sb_res)
                    else:
                        S.add("dve", "tensor_scalar", out=sbb[:, kc * 512:(kc + 1) * 512], in0=sf_ap[:, kc * 512:(kc + 1) * 512],
                              scalar1=gsc, scalar2=None, op0=ALU.mult, reads=sf_res + ["gainT"], writes=sb_res)
            elif pi % 2 == 0:
                S.add("act", "copy", out=sbb[:, 0:nk * 512], in_=sf_ap[:, 0:nk * 512], reads=sf_res, writes=sb_res)
            else:
                S.add("dve", "tensor_copy", out=sbb[:, 0:nk * 512], in_=sf_ap[:, 0:nk * 512], reads=sf_res, writes=sb_res)
            S.add("sp", "dma_start", out=wsc[b, :, 0:nk * 512], in_=sbb[:, 0:nk * 512],
                  reads=sb_res, writes=[("wsc", b)], dma_sem=f"D_ps{pi % NST}")

        for pi in range(min(NST - 1, len(ub))):
            pl_load(pi)
        for pi in range(len(ub)):
            if pi + NST - 1 < len(ub):
                pl_load(pi + NST - 1)
            pl_cast_store(pi)

        seq = []
        for t in range(ntiles):
            for b in range(NBLK):
                if b in used_blocks:
                    seq.append(b)
        wstate = {"next_load": 0, "next_use": 0}

        def wnext(hold=0):
            i = wstate["next_use"]
            wstate["next_use"] += 1
            while wstate["next_load"] < len(seq) and wstate["next_load"] <= i + NSLOT - 1 - hold:
                j = wstate["next_load"]
                b = seq[j]
                nk = WB[b][3]
                s = j % NSLOT
                S.add("sp", "dma_start", out=ws[s][:, 0:nk * 512], in_=wsc[b, :, 0:nk * 512],
                      reads=[("wsc", b)], writes=[("ws", s)], dma_sem=f"D_w{s}")
                wstate["next_load"] += 1
            s = i % NSLOT
            return ws[s], ("ws", s), seq[i]

        def rms_stats(tts):
            x_t = cur["x"]
            a, b_ = tts[0], tts[-1] + 1
            for tt in tts:
                S.add("act", "activation", out=osq[:, :], in_=x_t[:, tt * 1024:(tt + 1) * 1024],
                      func=AF.Square, accum_out=ss[:, tt:tt + 1],
                      reads=xres(tt), writes=[("osq", 0), ("osq", 1), ("ss", tt)])
            S.add("act", "activation", out=rs[:, a:b_], in_=ss[:, a:b_], func=AF.Ln,
                  scale=1.0 / D, bias=cst[:, 0:1], reads=[("ss", tt) for tt in tts] + ["cst"],
                  writes=[("rs", tt) for tt in tts])
            S.add("act", "activation", out=rs[:, a:b_], in_=rs[:, a:b_], func=AF.Exp, scale=-0.5,
                  reads=[("rs", tt) for tt in tts], writes=[("rs", tt) for tt in tts])

        def rms_stats_all():
            rms_stats([0, 1, 2, 3])

        def norm_to_hT_part(tts):
            x_t = cur["x"]
            rms_stats(tts)
            for p0 in range(0, len(tts), 2):
                pair = tts[p0:p0 + 2]
                for tt in pair:
                    hp = hpre[:, (tt % 2) * 1024:(tt % 2 + 1) * 1024]
                    if tt % 2 == 0:
                        S.add("act", "activation", out=hp, in_=x_t[:, tt * 1024:(tt + 1) * 1024], func=AF.Copy,
                              scale=rs[:, tt:tt + 1], reads=xres(tt) + [("rs", tt)], writes=[("hpre", tt % 2)])
                    else:
                        S.add("dve", "tensor_scalar", out=hp, in0=x_t[:, tt * 1024:(tt + 1) * 1024],
                              scalar1=rs[:, tt:tt + 1], scalar2=None, op0=ALU.mult,
                              reads=xres(tt) + [("rs", tt)], writes=[("hpre", tt % 2)])
                for tt in pair:
                    hp = hpre[:, (tt % 2) * 1024:(tt % 2 + 1) * 1024]
                    k = bank()
                    pb = ps[k][:, :].bitcast(BF16)
                    for kc in range(8):
                        S.add("pe", "transpose", out=pb[:, kc * 128:(kc + 1) * 128], in_=hp[:, kc * 128:(kc + 1) * 128],
                              identity=ident[:, :], reads=[("hpre", tt % 2), "ident"], writes=[("ps", k)])
                    dst = hT[:, :].rearrange("p (kc t) -> p kc t", t=512)[:, :, tt * 128:(tt + 1) * 128]
                    srcv = pb.rearrange("p (kc t) -> p kc t", t=128)
                    if tt % 2 == 0:
                        S.add("dve", "tensor_copy", out=dst, in_=srcv, reads=[("ps", k)], writes=[("hT", tt)])
                    else:
                        S.add("act", "copy", out=dst, in_=srcv, reads=[("ps", k)], writes=[("hT", tt)])

        def norm_to_hT(gi):
            norm_to_hT_part([0, 1, 2, 3])

        HT_ALL = [("hT", tt) for tt in range(4)]

        def proj_fm(wt, wres, j):
            k = bank()
            for kc in range(8):
                S.add("pe", "matmul", ps[k][:, :], lhsT=wt[:, kc * 512 + j * 128:kc * 512 + (j + 1) * 128],
                      rhs=hT[:, kc * 512:(kc + 1) * 512], start=(kc == 0), stop=(kc == 7),
                      reads=[wres] + HT_ALL, writes=[("ps", k)])
            return k

        def proj_tm(wt, wres, tt, src, src_res, nk=8, koff=0, k=None, first=True, last=True):
            if k is None:
                k = bank()
            for kc in range(nk):
                S.add("pe", "matmul", ps[k][:, :],
                      lhsT=src[:, (koff + kc) * 512 + tt * 128:(koff + kc) * 512 + (tt + 1) * 128],
                      rhs=wt[:, kc * 512:(kc + 1) * 512], start=(first and kc == 0), stop=(last and kc == nk - 1),
                      reads=[wres] + src_res, writes=[("ps", k)])
            return k

        def decay_tail(h, bsrc, bres, sign, dk_scale, eq_col, ek_dst, ek_res):
            eqk = cur["eqk"]
            S.add("act", "activation", out=eqk[:, eq_col:eq_col + 512], in_=bsrc, func=AF.Exp,
                  scale=sign * dk_scale, bias=cst[:, 2:3], reads=[bres, "cst"], writes=[eres(eq_col // 512)])
            S.add("act", "activation", out=ek_dst, in_=bsrc, func=AF.Exp, scale=-sign * dk_scale,
                  reads=[bres], writes=[ek_res])
            S.add("act", "activation", out=eblast[:, h * 8:(h + 1) * 8], in_=bsrc[:, 63:512:64], func=AF.Exp,
                  scale=sign * dk_scale, reads=[bres], writes=[("ebl", h)])

        def kd_scale(h):
            S.add("pool", "tensor_tensor", out=kdT_all[:, h * 512:(h + 1) * 512].rearrange("p (c t) -> p c t", t=64),
                  in0=kbog[:, h * 512:(h + 1) * 512].rearrange("p (c t) -> p c t", t=64),
                  in1=eblast[:, h * 8:(h + 1) * 8].unsqueeze(2).to_broadcast([128, 8, 64]), op=ALU.mult,
                  reads=[("kbog", h), ("ebl", h)], writes=[("act", h)])

        def kd_transpose(h):
            k = bank()
            pb = ps[k][:, :].bitcast(BF16)
            for tt in range(4):
                S.add("pe", "transpose", out=pb[:, tt * 128:(tt + 1) * 128],
                      in_=kdT_all[:, h * 512 + tt * 128:h * 512 + (tt + 1) * 128],
                      identity=ident[:, :], reads=[("act", h), "ident"], writes=[("ps", k)])
            S.add("dve", "tensor_copy",
                  out=kd_tok[:, :].rearrange("p (tt f) -> p tt f", f=1024)[:, :, h * 128:(h + 1) * 128],
                  in_=pb[:, 0:512].rearrange("p (tt f) -> p tt f", f=128),
                  reads=[("ps", k)], writes=[("kd_tok", h)])

        def attention_AT(H):
            AT_sb = cur["AT"]
            for h in range(H):
                k = bank()
                for tt in range(4):
                    S.add("pe", "matmul", ps[k][:, tt * 128:(tt + 1) * 128],
                          lhsT=kbog[:, h * 512 + tt * 128:h * 512 + (tt + 1) * 128],
                          rhs=qb[:, h * 512 + tt * 128:h * 512 + (tt + 1) * 128], start=True, stop=True,
                          reads=[("kbog", h), ("qb", h)], writes=[("ps", k)])
                S.add("dve", "tensor_tensor", out=AT_sb[:, h * 512:(h + 1) * 512], in0=ps[k][:, :], in1=mask4[:, :],
                      op=ALU.mult, reads=[("ps", k), "mask4"], writes=[eres(h // 2)])

        def attention_rec_init(tile_in_seq, S_f):
            if tile_in_seq == 0:
                S.add("dve", "memset", S_f[:, :], 0.0, writes=["S_f"])
            S.add("act", "copy", out=Sbf[0][:, :], in_=S_f[:, :], reads=["S_f"], writes=[("Sbf", 0)])

        def attention_rec_chunk(H, dv, S_f, c):
            hpb = 512 // dv
            tt, po = c // 2, (c % 2) * 64
            kA, kB = bank(), bank()
            for h in range(H):
                k = kA if h < hpb else kB
                col = (h % hpb) * dv
                S.add("pe", "matmul", ps[k][:, col:col + dv],
                      lhsT=kd_tok[po:po + 64, tt * 1024 + h * 128:tt * 1024 + (h + 1) * 128],
                      rhs=v_tok[po:po + 64, tt * 1024 + h * dv:tt * 1024 + (h + 1) * dv], start=True, stop=True,
                      reads=[("kd_tok", h), ("v_tok", tt)], writes=[("ps", k)])
            for h in range(H):
                k = kA if h < hpb else kB
                col = (h % hpb) * dv
                S.add("dve", "scalar_tensor_tensor", out=S_f[:, h * dv:(h + 1) * dv], in0=S_f[:, h * dv:(h + 1) * dv],
                      scalar=eblast[:, h * 8 + c:h * 8 + c + 1], in1=ps[k][:, col:col + dv],
                      op0=ALU.mult, op1=ALU.add,
                      reads=["S_f", ("ebl", h), ("ps", k)], writes=["S_f"])
            S.add("act", "copy", out=Sbf[c + 1][:, :], in_=S_f[:, :], reads=["S_f"], writes=[("Sbf", c + 1)])

        def attention_out(H, dv, ogn, ogn_res, groups, tok_rstd=False):
            nd = dv // 128
            AT_sb = cur["AT"]
            for g in groups:
                kS = bank(pin=True)
                for gi_, j in enumerate(g):
                    h = (j * 128) // dv
                    k = bank()
                    for tt in range(4):
                        S.add("pe", "matmul", ps[k][:, tt * 128:(tt + 1) * 128],
                              lhsT=v_tok[:, tt * 1024 + j * 128:tt * 1024 + (j + 1) * 128],
                              rhs=AT_sb[:, h * 512 + tt * 128:h * 512 + (tt + 1) * 128], start=True, stop=False,
                              reads=[("v_tok", tt), eres(h // 2)], writes=[("ps", k)])
                        for cc in range(2):
                            c = 2 * tt + cc
                            S.add("pe", "matmul", ps[k][:, c * 64:(c + 1) * 64],
                                  lhsT=Sbf[c][:, j * 128:(j + 1) * 128],
                                  rhs=qb[:, h * 512 + c * 64:h * 512 + (c + 1) * 64], start=False, stop=(cc == 1),
                                  reads=[("Sbf", c), ("qb", h)], writes=[("ps", k)])
                    if gi_ > 0:
                        jp = g[gi_ - 1]
                        S.add("pe", "matmul", ps[kS][:, :], lhsT=ones_bf[:, :], rhs=osq[:, (jp % 2) * 512:(jp % 2 + 1) * 512],
                              start=(gi_ - 1 == 0), stop=False,
                              reads=["ones_bf", ("osq", jp % 2)], writes=[("ps", kS)])
                    oq = osq[:, (j % 2) * 512:(j % 2 + 1) * 512]
                    S.add("act", "activation", out=oq, in_=ps[k][:, :], func=AF.Square,
                          reads=[("ps", k)], writes=[("osq", j % 2)])
                    oi = (j % nd) if nd > 1 else j
                    if tok_rstd:
                        S.add("dve", "scalar_tensor_tensor", out=kbog[:, j * 512:(j + 1) * 512], in0=ps[k][:, :],
                              scalar=ogn[:, oi:oi + 1],
                              in1=gate[:, j * 512:(j + 1) * 512], op0=ALU.mult, op1=ALU.mult,
                              reads=[("ps", k), ogn_res, ("gate", j), ("osq", j % 2)],
                              writes=[("kbog", j)])
                    else:
                        S.add("dve", "scalar_tensor_tensor", out=t1[:, j * 512:(j + 1) * 512], in0=ps[k][:, :],
                              scalar=ogn[:, oi:oi + 1],
                              in1=gate[:, j * 512:(j + 1) * 512], op0=ALU.mult, op1=ALU.mult,
                              reads=[("ps", k), ogn_res, ("gate", j), ("osq", j % 2)],
                              writes=[("act", 2 * j), ("act", 2 * j + 1)])
                    if gi_ == len(g) - 1:
                        S.add("pe", "matmul", ps[kS][:, :], lhsT=ones_bf[:, :], rhs=oq,
                              start=(gi_ == 0), stop=True,
                              reads=["ones_bf", ("osq", j % 2)], writes=[("ps", kS)])
                S.add("act", "activation", out=rstd_t[:, :], in_=ps[kS][:, :], func=AF.Ln,
                      scale=1.0 / (128 * len(g)), bias=cst[:, 0:1], reads=[("ps", kS), "cst"], writes=["rstd_t"])
                S.add("act", "activation", out=rstd_t[:, :], in_=rstd_t[:, :], func=AF.Exp, scale=-0.5,
                      reads=["rstd_t"], writes=["rstd_t"])
                pinned.discard(kS)
                if tok_rstd:
                    continue
                for ji, j in enumerate(g):
                    eng = "dve" if ji % 2 == 0 else "pool"
                    S.add(eng, "tensor_tensor", out=kbog[:, j * 512:(j + 1) * 512], in0=t1[:, j * 512:(j + 1) * 512],
                          in1=rstd_t[:, :], op=ALU.mult,
                          reads=[("act", 2 * j), ("act", 2 * j + 1), "rstd_t"], writes=[("kbog", j)])

        def residual_add(k, tt, ob):
            x_t = cur["x"]
            S.add("dve", "tensor_tensor", out=x_t[:, tt * 1024 + ob * 512:tt * 1024 + (ob + 1) * 512],
                  in0=x_t[:, tt * 1024 + ob * 512:tt * 1024 + (ob + 1) * 512], in1=ps[k][:, :], op=ALU.add,
                  reads=xres(tt) + [("ps", k)], writes=xres(tt))

        def out_proj_residual(tok_rstd=False):
            OG = [("kbog", j) for j in range(8)]
            x_t = cur["x"]
            w = [wnext(), wnext(hold=1)]

            def mm(tts):
                out = []
                for ob in range(2):
                    for tt in tts:
                        out.append((proj_tm(w[ob][0], w[ob][1], tt, kbog, OG), tt, ob))
                return out

            def adds(lst):
                for k, tt, ob in lst:
                    if tok_rstd:
                        xs = x_t[:, tt * 1024 + ob * 512:tt * 1024 + (ob + 1) * 512]
                        S.add("dve", "scalar_tensor_tensor", out=xs, in0=ps[k][:, :], scalar=rsd_tok[:, tt:tt + 1],
                              in1=xs, op0=ALU.mult, op1=ALU.add,
                              reads=xres(tt) + [("ps", k), "rsd_tok"], writes=xres(tt))
                    else:
                        residual_add(k, tt, ob)

            h0 = mm([0, 1])
            if tok_rstd:
                kR = bank()
                for t2 in range(4):
                    S.add("pe", "matmul", ps[kR][:, t2:t2 + 1], lhsT=rstd_t[0:1, t2 * 128:(t2 + 1) * 128],
                          rhs=cst[0:1, 1:2], start=True, stop=True,
                          reads=["rstd_t", "cst"], writes=[("ps", kR)])
                S.add("act", "copy", out=rsd_tok[:, 0:4], in_=ps[kR][:, 0:4], reads=[("ps", kR)], writes=["rsd_tok"])
            adds(h0)
            h1 = mm([2, 3])
            norm_to_hT_part([0, 1])
            adds(h1)
            norm_to_hT_part([2, 3])

        def ffn_in(layer, do_norm=True):
            if do_norm:
                norm_to_hT(2 + layer)
            for j in range(11):
                wt, wres, _ = wnext()
                for ch in range(2):
                    kg = proj_fm(wt, wres, ch)
                    ku = proj_fm(wt, wres, 2 + ch)
                    sgt = sg[:, (ch % 2) * 512:(ch % 2 + 1) * 512]
                    S.add("act", "activation", out=sgt, in_=ps[kg][:, :], func=AF.Silu,
                          reads=[("ps", kg)], writes=[("sg", ch % 2)])
                    cidx = 2 * j + ch
                    S.add("dve", "tensor_tensor", out=act[:, cidx * 512:(cidx + 1) * 512], in0=ps[ku][:, :], in1=sgt,
                          op=ALU.mult, reads=[("ps", ku), ("sg", ch % 2)], writes=[("act", cidx)])

        def ffn_out(layer, between=None):
            ACT_ALL = [("act", c) for c in range(22)]
            for ob in range(2):
                if ob == 1 and between is not None:
                    between()
                ks = [bank() for _ in range(4)]
                for kr in range(3):
                    wt, wres, b = wnext()
                    nk = WB[b][3]
                    for tt in range(4):
                        proj_tm(wt, wres, tt, act, ACT_ALL, nk=nk, koff=8 * kr, k=ks[tt], first=(kr == 0), last=(kr == 2))
                for tt in range(4):
                    residual_add(ks[tt], tt, ob)

        def v_proj(vbs=(0, 1)):
            for vb in vbs:
                wt, wres, _ = wnext()
                for tt in range(4):
                    k = proj_tm(wt, wres, tt, hT, HT_ALL)
                    dst = v_tok[:, tt * 1024 + vb * 512:tt * 1024 + (vb + 1) * 512]
                    S.add("dve", "tensor_copy", out=dst, in_=ps[k][:, :], reads=[("ps", k)], writes=[("v_tok", tt)])

        def gate_groups():
            out = []
            for rb in range(2):
                holder = {}
                for jj in range(4):
                    def g(rb=rb, jj=jj, holder=holder):
                        if jj == 0:
                            holder["w"] = wnext()
                        wt, wres, _ = holder["w"]
                        k = proj_fm(wt, wres, jj)
                        j = rb * 4 + jj
                        S.add("act", "activation", out=gate[:, j * 512:(j + 1) * 512], in_=ps[k][:, :], func=AF.Silu,
                              reads=[("ps", k)], writes=[("gate", j)])
                    out.append(g)
            return out

        def gla_q_groups():
            eqk = cur["eqk"]
            out = []
            holder = {}
            for h in range(4):
                def g(h=h, holder=holder):
                    if h == 0:
                        holder["w"] = wnext()
                    wt, wres, _ = holder["w"]
                    k = proj_fm(wt, wres, h)
                    S.add("dve", "tensor_tensor", out=qb[:, h * 512:(h + 1) * 512], in0=ps[k][:, :],
                          in1=eqk[:, h * 512:(h + 1) * 512], op=ALU.mult,
                          reads=[("ps", k), eres(h)], writes=[("qb", h)])
                out.append(g)
            return out

        def hgrn_q_groups():
            eqk = cur["eqk"]
            out = []
            for qblk in range(2):
                holder = {}
                for hh in range(4):
                    def g(qblk=qblk, hh=hh, holder=holder):
                        if hh == 0:
                            holder["w"] = wnext()
                        wt, wres, _ = holder["w"]
                        h = qblk * 4 + hh
                        k = proj_fm(wt, wres, hh)
                        ta = gL[:, (h % 2) * 512:(h % 2 + 1) * 512]
                        S.add("act", "activation", out=ta, in_=ps[k][:, :], func=AF.Silu,
                              reads=[("ps", k)], writes=[("gL", h % 2)])
                        S.add("dve", "tensor_tensor", out=qb[:, h * 512:(h + 1) * 512], in0=ta,
                              in1=eqk[:, h * 512:(h + 1) * 512], op=ALU.mult,
                              reads=[("gL", h % 2), eres(h)], writes=[("qb", h)])
                    out.append(g)
            return out

        def gla_layer(tile_in_seq, after_attention, do_norm=True):
            eqk = cur["eqk"]
            if do_norm:
                norm_to_hT(0)
            k = bank()
            for kc in range(8):
                S.add("pe", "matmul", ps[k][0:16, :], lhsT=wgd[:, kc * 16:(kc + 1) * 16], rhs=hT[:, kc * 512:(kc + 1) * 512],
                      start=(kc == 0), stop=(kc == 7), reads=["wgd"] + HT_ALL, writes=[("ps", k)])
            S.add("act", "copy", out=gd_sb[0:16, :], in_=ps[k][0:16, :], reads=[("ps", k)], writes=["gd_sb"])

            def stage1(h):
                k = bank()
                S.add("pe", "matmul", ps[k][:, :], lhsT=wgu[0:16, h * 128:(h + 1) * 128], rhs=gd_sb[0:16, :],
                      start=True, stop=True, reads=["wgu", "gd_sb"], writes=[("ps", k)])
                ta = tmpA[:, (h % 3) * 512:(h % 3 + 1) * 512]
                S.add("act", "activation", out=ta, in_=ps[k][:, :], func=AF.Exp, scale=-1.0, bias=negbg[:, h:h + 1],
                      reads=[("ps", k), "negbg"], writes=[("tmpA", h % 3)])
                gl = gL[:, (h % 2) * 512:(h % 2 + 1) * 512]
                S.add("act", "activation", out=gl, in_=ta, func=AF.Ln, scale=1.0, bias=cst[:, 1:2],
                      reads=[("tmpA", h % 3), "cst"], writes=[("gL", h % 2)])
                bc = bcr[:, (h % 3) * 512:(h % 3 + 1) * 512]
                S.add("dve", "tensor_tensor_scan", out=bc, data0=maskc[:, :], data1=gl, initial=0.0,
                      op0=ALU.mult, op1=ALU.add, reads=["maskc", ("gL", h % 2)], writes=[("bcr", h % 3)])

            def stage2(h):
                bc = bcr[:, (h % 3) * 512:(h % 3 + 1) * 512]
                decay_tail(h, bc, ("bcr", h % 3), -1.0, 1.0 / 16.0, h * 512,
                           eqk[:, 2048 + h * 512:2048 + (h + 1) * 512], eres(4 + h))

            v_proj((0,))
            for h in range(4):
                stage1(h)
                if h >= 1:
                    stage2(h - 1)
            stage2(3)
            v_proj((1,))
            wt, wres, _ = wnext()
            for h in range(4):
                k = proj_fm(wt, wres, h)
                S.add("dve", "tensor_tensor", out=kbog[:, h * 512:(h + 1) * 512], in0=ps[k][:, :],
                      in1=eqk[:, 2048 + h * 512:2048 + (h + 1) * 512], op=ALU.mult,
                      reads=[("ps", k), eres(4 + h)], writes=[("kbog", h)])
                kd_scale(h)
            for h in range(4):
                kd_transpose(h)
            groups = gate_groups() + gla_q_groups()
            attention_rec_init(tile_in_seq, S_fs[0])
            gi = 0
            for c in range(NCH):
                attention_rec_chunk(4, 256, S_fs[0], c)
                n = 2 if c < 4 else 1
                for _ in range(n):
                    groups[gi]()
                    gi += 1
            assert gi == len(groups)
            attention_AT(4)
            attention_out(4, 256, ognorm0, "ognorm0", [[0, 1], [2, 3], [4, 5], [6, 7]])
            after_attention()
            out_proj_residual()

        def hgrn_layer(tile_in_seq, after_attention, do_norm=True):
            eqk = cur["eqk"]
            if do_norm:
                norm_to_hT(1)
            fw = {}

            def stage1(h):
                if h % 4 == 0:
                    fw["w"] = wnext()
                wt, wres, _ = fw["w"]
                k = proj_fm(wt, wres, h % 4)
                ta = tmpA[:, (h % 3) * 512:(h % 3 + 1) * 512]
                gl = gL[:, (h % 2) * 512:(h % 2 + 1) * 512]
                l1 = sg[:, (h % 2) * 512:(h % 2 + 1) * 512]
                S.add("act", "activation", out=ta, in_=ps[k][:, :], func=AF.Exp, scale=-1.0,
                      reads=[("ps", k)], writes=[("tmpA", h % 3)])
                S.add("act", "activation", out=l1, in_=ta, func=AF.Ln, scale=1.0, bias=cst[:, 1:2],
                      reads=[("tmpA", h % 3), "cst"], writes=[("sg", h % 2)])
                S.add("act", "activation", out=gl, in_=ta, func=AF.Ln, scale=lb_t[:, h:h + 1], bias=cst[:, 1:2],
                      reads=[("tmpA", h % 3), "cst", "lb"], writes=[("gL", h % 2)])
                S.add("dve", "tensor_tensor", out=gl, in0=gl, in1=l1, op=ALU.subtract,
                      reads=[("gL", h % 2), ("sg", h % 2)], writes=[("gL", h % 2)])
                bc = bcr[:, (h % 3) * 512:(h % 3 + 1) * 512]
                S.add("dve", "tensor_tensor_scan", out=bc, data0=maskc[:, :], data1=gl, initial=0.0,
                      op0=ALU.mult, op1=ALU.add, reads=["maskc", ("gL", h % 2)], writes=[("bcr", h % 3)])
                S.add("act", "activation", out=ta, in_=l1, func=AF.Exp, scale=-1.0,
                      reads=[("sg", h % 2)], writes=[("tmpA", h % 3)])
                S.add("dve", "tensor_scalar", out=ta, in0=ta, scalar1=noml[:, h:h + 1], scalar2=oml[:, h:h + 1],
                      op0=ALU.mult, op1=ALU.add, reads=[("tmpA", h % 3), "noml", "oml"], writes=[("tmpA", h % 3)])

            def stage2(h):
                ta = tmpA[:, (h % 3) * 512:(h % 3 + 1) * 512]
                bc = bcr[:, (h % 3) * 512:(h % 3 + 1) * 512]
                ekt = sg[:, (h % 2) * 512:(h % 2 + 1) * 512]
                decay_tail(h, bc, ("bcr", h % 3), 1.0, 1.0, h * 512, ekt, ("sg", h % 2))
                S.add("dve", "tensor_tensor", out=kbog[:, h * 512:(h + 1) * 512], in0=ta, in1=ekt, op=ALU.mult,
                      reads=[("tmpA", h % 3), ("sg", h % 2)], writes=[("kbog", h)])
                kd_scale(h)

            for h in range(8):
                stage1(h)
                if h >= 1:
                    stage2(h - 1)
            stage2(7)
            v_proj()
            for h in range(8):
                kd_transpose(h)
            groups = hgrn_q_groups() + gate_groups()
            attention_rec_init(tile_in_seq, S_fs[1])
            gi = 0
            for c in range(NCH):
                attention_rec_chunk(8, 128, S_fs[1], c)
                for _ in range(2):
                    groups[gi]()
                    gi += 1
            assert gi == len(groups)
            attention_AT(8)
            attention_out(8, 128, ognorm1, "ognorm1", [list(range(8))], tok_rstd=True)
            after_attention()
            out_proj_residual(tok_rstd=True)

        def load_x(t):
            p = t % 2
            S.add("pool", "dma_start", out=XB[p][:, :].rearrange("p (tt d) -> p tt d", d=1024),
                  in_=x_in[t * T:(t + 1) * T, :].rearrange("(tt p) d -> p tt d", p=128),
                  writes=xall(p), dma_sem=f"D_x{p}")

        load_x(0)
        last_layer = max(layers)
        first_layer = min(layers)
        set_tile_bufs(0)
        norm_to_hT(first_layer)
        for t in range(ntiles):
            tis = t % tiles_per_seq
            set_tile_bufs(t)

            def prefetch_next(t=t):
                if t + 1 < ntiles:
                    load_x(t + 1)

            def nop():
                pass

            def pre_norm_next(t=t):
                if t + 1 < ntiles:
                    set_tile_bufs(t + 1)
                    norm_to_hT(first_layer)
                    set_tile_bufs(t)

            if 0 in layers:
                gla_layer(tis, prefetch_next if last_layer == 0 else nop, do_norm=False)
                ffn_in(0, do_norm=False)
                ffn_out(0, between=(pre_norm_next if last_layer == 0 else None))
            if 1 in layers:
                hgrn_layer(tis, prefetch_next if last_layer == 1 else nop, do_norm=(first_layer != 1))
                ffn_in(1, do_norm=False)
                ffn_out(1, between=pre_norm_next)
            x_t = cur["x"]
            if final_norm:
                rms_stats_all()
                for tt in range(4):
                    S.add("dve", "scalar_tensor_tensor", out=x_t[:, tt * 1024:(tt + 1) * 1024],
                          in0=x_t[:, tt * 1024:(tt + 1) * 1024], scalar=rs[:, tt:tt + 1], in1=gfin[:, :],
                          op0=ALU.mult, op1=ALU.mult, reads=xres(tt) + [("rs", tt), "gfin"], writes=xres(tt))
            S.add("pool", "dma_start", out=y_out[t * T:(t + 1) * T, :].rearrange("(tt p) d -> p tt d", p=128),
                  in_=x_t[:, :].rearrange("p (tt d) -> p tt d", d=1024),
                  reads=xall(), writes=[("y", t)], dma_sem=f"D_y{t % 2}")
        S.add("pool", None, reads=[("y", t) for t in range(ntiles)])
        S.emit(nc, es)
    return nc


_PARAM_NAMES = ["mixer_norm", "ffn_norm", "gla_w_in", "gla_w_gate_up", "gla_b_gate", "gla_head_norm",
                "gla_w_out", "hgrn_w_in", "hgrn_lower_bounds", "hgrn_out_norm", "hgrn_w_out",
                "ffn_w_in", "ffn_w_out", "final_norm"]


def run_module(inputs, layers=(0, 1), final_norm=True):
    x = np.ascontiguousarray(np.asarray(inputs["x"], dtype=np.float32))
    B, SEQ, _ = x.shape
    assert B % NCORES == 0 and SEQ % T == 0
    bpc = B // NCORES
    tiles_per_seq = SEQ // T
    ntiles = bpc * tiles_per_seq
    nc = build_program(ntiles, tiles_per_seq, layers=layers, final_norm=final_norm)
    params = {k: np.ascontiguousarray(np.asarray(inputs[k], dtype=np.float32)) for k in _PARAM_NAMES}
    in_maps = []
    for c in range(NCORES):
        m = {"x": x[c * bpc:(c + 1) * bpc].reshape(bpc * SEQ, D)}
        m.update(params)
        in_maps.append(m)
    res = run_bass_kernel_spmd(nc, in_maps, core_ids=list(range(NCORES)))
    out = np.concatenate([np.asarray(r["y"]).reshape(bpc, SEQ, D) for r in res.results], axis=0)
    return out.astype(np.float32)


def kernel(**inputs):
    return run_module(inputs)
```

```python
import math
from contextlib import ExitStack

import numpy as np
import concourse.bass as bass
import concourse.mybir as mybir
from concourse.bass_utils import run_bass_kernel_spmd

F32 = mybir.dt.float32
BF16 = mybir.dt.bfloat16
AF = mybir.ActivationFunctionType
ALU = mybir.AluOpType

D = 1024
DFF = 2816
T = 512
NTT = 4
NCH = 8
EPS = 1e-6
NSLOT = 5
NCORES = 8


class Op:
    __slots__ = ("eng", "meth", "args", "kw", "deps", "dma_sem", "sem", "val", "inc", "waits")


class Sched:
    def __init__(self):
        self.ops = []
        self.last_writer = {}
        self.readers = {}
        self.last_dma = {}

    def add(self, eng, meth, *args, reads=(), writes=(), dma_sem=None, **kw):
        idx = len(self.ops)
        deps = []
        lw = self.last_writer
        for r in reads:
            w = lw.get(r)
            if w is not None:
                deps.append((w, 0))
        for r in writes:
            w = lw.get(r)
            if w is not None:
                deps.append((w, 1))
            for rd in self.readers.get(r, ()):
                deps.append((rd, 1))
        if dma_sem is not None:
            pd = self.last_dma.get(dma_sem)
            if pd is not None:
                deps.append((pd, 0))
            self.last_dma[dma_sem] = idx
        for r in reads:
            self.readers.setdefault(r, []).append(idx)
        for r in writes:
            lw[r] = idx
            self.readers[r] = []
        op = Op()
        op.eng, op.meth, op.args, op.kw = eng, meth, args, kw
        op.deps, op.dma_sem = deps, dma_sem
        self.ops.append(op)
        return idx

    @staticmethod
    def _skip(p, c, kind):
        if p.eng == c.eng and p.dma_sem is None and c.dma_sem is None:
            if p.eng == "pe":
                return True
        return False

    def resolve(self):
        ops = self.ops
        needed = [False] * len(ops)
        for op in ops:
            for d, kind in op.deps:
                if not self._skip(ops[d], op, kind):
                    needed[d] = True
        cnt = {}
        for i, op in enumerate(ops):
            if op.dma_sem is not None:
                op.sem = op.dma_sem
                cnt[op.sem] = cnt.get(op.sem, 0) + 16
                op.val, op.inc = cnt[op.sem], 16
            elif needed[i] and op.meth is not None:
                op.sem = "E_" + op.eng
                cnt[op.sem] = cnt.get(op.sem, 0) + 1
                op.val, op.inc = cnt[op.sem], 1
            else:
                op.sem, op.val, op.inc = None, 0, 0
        eng_clock = {}
        done = [None] * len(ops)
        for i, op in enumerate(ops):
            clk = eng_clock.setdefault(op.eng, {})
            waits = {}
            for d, kind in op.deps:
                p = ops[d]
                if self._skip(p, op, kind):
                    continue
                if clk.get(p.sem, 0) >= p.val:
                    continue
                if waits.get(p.sem, 0) < p.val:
                    waits[p.sem] = p.val
                for k, v in done[d].items():
                    if clk.get(k, 0) < v:
                        clk[k] = v
            op.waits = list(waits.items())
            if op.sem is not None:
                dc = dict(clk)
                dc[op.sem] = op.val
                done[i] = dc
        return cnt

    def emit(self, nc, es):
        cnt = self.resolve()
        sems = {}
        for name in sorted(cnt):
            sems[name] = es.enter_context(nc.semaphore(name))
        per_eng = {}
        for op in self.ops:
            per_eng.setdefault(op.eng, []).append(op)

        def run(engname, e):
            for op in per_eng.get(engname, ()):
                for s, v in op.waits:
                    e.wait_ge(sems[s], v)
                if op.meth is None:
                    continue
                ins = getattr(e, op.meth)(*op.args, **op.kw)
                if op.inc:
                    ins.then_inc(sems[op.sem], op.inc)

        block = es.enter_context(nc.Block())

        @block.tensor
        def _(e):
            run("pe", e)

        @block.scalar
        def _(e):
            run("act", e)

        @block.vector
        def _(e):
            run("dve", e)

        @block.gpsimd
        def _(e):
            run("pool", e)

        @block.sync
        def _(e):
            run("sp", e)


def weight_blocks():
    blks = []
    for c0 in (1024, 1536, 512, 2048, 2560, 0):
        blks.append(("gla_w_in", 0, 0, 8, [(c0, 512)]))
    for c0 in (0, 512):
        blks.append(("gla_w_out", 0, 0, 8, [(c0, 512)]))
    for layer in (0, 1):
        if layer == 1:
            for c0 in (1024, 1536, 2048, 2560, 0, 512, 3072, 3584):
                blks.append(("hgrn_w_in", 0, 0, 8, [(c0, 512)]))
            for c0 in (0, 512):
                blks.append(("hgrn_w_out", 0, 0, 8, [(c0, 512)]))
        for j in range(11):
            blks.append(("ffn_w_in", layer, 0, 8, [(256 * j, 256), (DFF + 256 * j, 256)]))
        for ob in range(2):
            for kr in range(3):
                nk = 8 if kr < 2 else 6
                blks.append(("ffn_w_out", layer, 8 * kr, nk, [(512 * ob, 512)]))
    return blks


WB = weight_blocks()
NBLK = len(WB)


def build_program(ntiles, tiles_per_seq, layers=(0, 1), final_norm=True):
    ntok = ntiles * T
    nc = bass.Bass("TRN2", target_bir_lowering=False)
    S = Sched()

    def din(name, shape):
        return nc.dram_tensor(name, list(shape), F32, kind="ExternalInput").ap()

    x_in = din("x", (ntok, D))
    dram = {
        "mixer_norm": din("mixer_norm", (2, D)),
        "ffn_norm": din("ffn_norm", (2, D)),
        "gla_w_in": din("gla_w_in", (1, D, 3088)),
        "gla_w_gate_up": din("gla_w_gate_up", (1, 16, 512)),
        "gla_b_gate": din("gla_b_gate", (1, 512)),
        "gla_head_norm": din("gla_head_norm", (1, 256)),
        "gla_w_out": din("gla_w_out", (1, D, D)),
        "hgrn_w_in": din("hgrn_w_in", (1, D, 4096)),
        "hgrn_lower_bounds": din("hgrn_lower_bounds", (2, D)),
        "hgrn_out_norm": din("hgrn_out_norm", (1, D)),
        "hgrn_w_out": din("hgrn_w_out", (1, D, D)),
        "ffn_w_in": din("ffn_w_in", (2, D, 2 * DFF)),
        "ffn_w_out": din("ffn_w_out", (2, DFF, D)),
        "final_norm": din("final_norm", (D,)),
    }
    y_out = nc.dram_tensor("y", [ntok, D], F32, kind="ExternalOutput").ap()
    wsc = nc.dram_tensor("wsc", [NBLK, 128, 4096], BF16, kind="Internal").ap()

    es = ExitStack()
    with es:
        def sb(name, cols, dt, parts=128):
            return es.enter_context(nc.sbuf_tensor(name, [parts, cols], dt))

        XB = [sb("XB0", 4096, F32), sb("XB1", 4096, F32)]
        hpre = sb("hpre", 2048, BF16)
        hT = sb("hT", 4096, BF16)
        wsbig = sb("wsbig", NSLOT * 4096, BF16)
        ws = [wsbig[:, i * 4096:(i + 1) * 4096] for i in range(NSLOT)]
        gL = sb("gL", 1024, F32)
        bcr = sb("bcr", 1536, F32)
        qb = sb("qb", 4096, BF16)
        kbog = sb("kbog", 4096, BF16)
        kd_tok = sb("kd_tok", 4096, BF16)
        v_tok = sb("v_tok", 4096, BF16)
        gate = sb("gate", 4096, BF16)
        S_fs = [sb(f"S_f{i}", 1024, F32) for i in range(2)]
        Sbf = [sb(f"Sbf{i}", 1024, BF16) for i in range(9)]
        act = sb("act", 22 * 512, BF16)
        osq = sb("osq", 1024, BF16)
        rstd_t = sb("rstd_t", 512, F32)
        tmpA = sb("tmpA", 1536, F32)
        sg = sb("sg", 1024, F32)
        onesf = tmpA[:, 0:128]
        wgu_f = sg[:, 0:512]
        wgd_f = sg[:, 512:640]
        maskc = sb("maskc", 512, F32)
        mask4 = sb("mask4", 512, BF16)
        ident = sb("ident", 128, BF16)
        ones_bf = sb("ones_bf", 128, BF16)
        gainT = sb("gainT", 40, F32)
        ognorm0 = sb("ognorm0", 2, F32)
        ognorm1 = sb("ognorm1", 8, F32)
        negbg = sb("negbg", 4, F32)
        hb = sb("hb", 16, F32)
        lb_t = sb("lb_t", 8, F32)
        oml = sb("oml", 8, F32)
        noml = sb("noml", 8, F32)
        gfin = sb("gfin", 1024, F32)
        wgd = sb("wgd", 128, BF16)
        wgu = sb("wgu", 512, BF16)
        gd_sb = sb("gd_sb", 512, BF16)
        ss = sb("ss", 4, F32)
        rs = sb("rs", 4, F32)
        rsd_tok = sb("rsd_tok", 4, F32)
        eblast = sb("eblast", 64, F32)
        cst = sb("cst", 4, F32)
        ps = [es.enter_context(nc.psum_tensor(f"ps{i}", [128, 512], F32)) for i in range(8)]

        t1 = act[:, 0:8192].bitcast(F32)
        kdT_all = act

        cur = {}

        def set_tile_bufs(t):
            p = t % 2
            cur["xp"], cur["ep"] = p, 1 - p
            cur["x"] = XB[p]
            cur["eqk"] = XB[1 - p]
            cur["AT"] = XB[1 - p][:, 0:2048].bitcast(BF16)

        def xres(tt, p=None):
            p = cur["xp"] if p is None else p
            return [("XB", p, 2 * tt), ("XB", p, 2 * tt + 1)]

        def xall(p=None):
            p = cur["xp"] if p is None else p
            return [("XB", p, g) for g in range(8)]

        def eres(g):
            return ("XB", cur["ep"], g)

        pstate = {"i": 0}
        pinned = set()

        def bank(pin=False):
            while True:
                k = pstate["i"] % 8
                pstate["i"] += 1
                if k not in pinned:
                    break
            if pin:
                pinned.add(k)
            return k

        csem = "D_const"

        def cload(out_ap, in_ap, res):
            S.add("pool", "dma_start", out=out_ap, in_=in_ap, allow_slow_non_contiguous=True,
                  writes=[res], dma_sem=csem)

        S.add("dve", "memset", cst[:, 0:1], EPS, writes=["cst"])
        S.add("dve", "memset", cst[:, 1:2], 1.0, writes=["cst"])
        S.add("dve", "memset", cst[:, 2:3], math.log(128 ** -0.5), writes=["cst"])
        S.add("dve", "memset", cst[:, 3:4], 0.0, writes=["cst"])
        S.add("dve", "memset", maskc[:, :], 1.0, writes=["maskc"])
        S.add("dve", "memset", maskc[:, 0:512:64], 0.0, writes=["maskc"])
        S.add("dve", "memset", onesf, 1.0, writes=[("tmpA", 0)])
        S.add("dve", "tensor_copy", out=ones_bf[:, :], in_=onesf, reads=[("tmpA", 0)], writes=["ones_bf"])
        S.add("pool", "affine_select", out=ident[:, :], in_=ones_bf[:, :], pattern=[[-1, 128]],
              compare_op=ALU.is_equal, fill=0.0, base=0, channel_multiplier=1,
              reads=["ones_bf"], writes=["ident"])
        S.add("pool", "affine_select", out=mask4[:, 0:128], in_=onesf, pattern=[[1, 128]],
              compare_op=ALU.is_ge, fill=0.0, base=0, channel_multiplier=-1,
              reads=[("tmpA", 0)], writes=["mask4"])
        S.add("pool", "memset", mask4[0:64, 64:128], 0.0, writes=["mask4"])
        for r in range(1, 4):
            S.add("pool", "tensor_copy", out=mask4[:, r * 128:(r + 1) * 128], in_=mask4[:, 0:128],
                  reads=["mask4"], writes=["mask4"])

        for li in range(2):
            cload(gainT[:, li * 8:(li + 1) * 8], dram["mixer_norm"][li, :].rearrange("(kc p) -> p kc", p=128), "gainT")
            cload(gainT[:, 16 + li * 8:16 + (li + 1) * 8], dram["ffn_norm"][li, :].rearrange("(kc p) -> p kc", p=128), "gainT")
        cload(ognorm0[:, :], dram["gla_head_norm"][0, :].rearrange("(kc p) -> p kc", p=128), "ognorm0")
        cload(ognorm1[:, :], dram["hgrn_out_norm"][0, :].rearrange("(kc p) -> p kc", p=128), "ognorm1")
        cload(negbg[:, :], dram["gla_b_gate"][0, :].rearrange("(kc p) -> p kc", p=128), "negbg")
        S.add("dve", "tensor_scalar", out=negbg[:, :], in0=negbg[:, :], scalar1=-1.0, scalar2=None, op0=ALU.mult,
              reads=["negbg"], writes=["negbg"])
        for li in range(2):
            cload(hb[:, li * 8:(li + 1) * 8], dram["hgrn_lower_bounds"][li, :].rearrange("(kc p) -> p kc", p=128), "hb")
        S.add("dve", "tensor_tensor", out=lb_t[:, :], in0=hb[:, 8:16], in1=hb[:, 0:8], op=ALU.subtract,
              reads=["hb"], writes=["lb"])
        S.add("act", "activation", out=lb_t[:, :], in_=lb_t[:, :], func=AF.Sigmoid, reads=["lb"], writes=["lb"])
        S.add("dve", "tensor_scalar", out=oml[:, :], in0=lb_t[:, :], scalar1=-1.0, scalar2=1.0, op0=ALU.mult, op1=ALU.add,
              reads=["lb"], writes=["oml"])
        S.add("dve", "tensor_scalar", out=noml[:, :], in0=lb_t[:, :], scalar1=-1.0, scalar2=None, op0=ALU.add,
              reads=["lb"], writes=["noml"])
        cload(gfin[:, :], dram["final_norm"].partition_broadcast(128), "gfin")
        cload(wgd_f.rearrange("p (kc f) -> p kc f", f=16),
              dram["gla_w_in"][0, :, 3072:3088].rearrange("(kc p) f -> p kc f", p=128), ("sg", 1))
        for kc in range(8):
            S.add("dve", "tensor_scalar", out=wgd[:, kc * 16:(kc + 1) * 16], in0=wgd_f[:, kc * 16:(kc + 1) * 16],
                  scalar1=gainT[:, kc:kc + 1], scalar2=None, op0=ALU.mult,
                  reads=[("sg", 1), "gainT"], writes=["wgd"])
        cload(wgu_f[0:16, :], dram["gla_w_gate_up"][0, :, :], ("sg", 0))
        S.add("dve", "tensor_copy", out=wgu[0:16, :], in_=wgu_f[0:16, :], reads=[("sg", 0)], writes=["wgu"])

        stage_f = [(XB[0][:, :], [("XB", 0, g) for g in range(8)]),
                   (XB[1][:, :], [("XB", 1, g) for g in range(8)]),
                   (act[:, 0:8192].bitcast(F32), [("act", c) for c in range(16)]),
                   (wsbig[:, 0:8192].bitcast(F32), [("ws", 0), ("ws", 1)]),
                   (wsbig[:, 8192:16384].bitcast(F32), [("ws", 2), ("ws", 3)])]
        stage_b = [(hT[:, :], [("hT", tt) for tt in range(4)]), (qb[:, :], [("qb", h) for h in range(8)]),
                   (kbog[:, :], [("kbog", h) for h in range(8)]), (kd_tok[:, :], [("kd_tok", h) for h in range(8)]),
                   (v_tok[:, :], [("v_tok", tt) for tt in range(4)])]
        NST = len(stage_f)
        used_blocks = set()
        for b in range(NBLK):
            lay = 0 if b < 25 else 1
            if lay in layers:
                used_blocks.add(b)
        ub = sorted(used_blocks)

        def pl_load(pi):
            src, li, k0, nk, cols = WB[ub[pi]]
            sf_ap, sf_res = stage_f[pi % NST]
            c0 = 0
            for (col, w) in cols:
                S.add("sp", "dma_start",
                      out=sf_ap[:, 0:nk * 512].rearrange("p (kc f) -> p kc f", f=512)[:, :, c0:c0 + w],
                      in_=dram[src][li, k0 * 128:(k0 + nk) * 128, col:col + w].rearrange("(kc p) f -> p kc f", p=128),
                      writes=sf_res, dma_sem=f"D_pl{pi % NST}")
                c0 += w

        def pl_cast_store(pi):
            b = ub[pi]
            nk = WB[b][3]
            sf_ap, sf_res = stage_f[pi % NST]
            sbb, sb_res = stage_b[pi % NST]
            src, li = WB[b][0], WB[b][1]
            gi = {"gla_w_in": 0, "hgrn_w_in": 1}.get(src, (2 + li) if src == "ffn_w_in" else None)
            if gi is not None:
                for kc in range(nk):
                    gsc = gainT[:, gi * 8 + kc:gi * 8 + kc + 1]
                    if pi % 2 == 0:
                        S.add("act", "activation", out=sbb[:, kc * 512:(kc + 1) * 512], in_=sf_ap[:, kc * 512:(kc + 1) * 512],
                              func=AF.Copy, scale=gsc, reads=sf_res + ["gainT"], writes=sb_res)
                    else:
                        S.add("dve", "tensor_scalar", out=sbb[:, kc * 512:(kc + 1) * 512], in0=sf_ap[:, kc * 512:(kc + 1) * 512],
                              scalar1=gsc, scalar2=None, op0=ALU.mult, reads=sf_res + ["gainT"], writes=sb_res)
            elif pi % 2 == 0:
                S.add("act", "copy", out=sbb[:, 0:nk * 512], in_=sf_ap[:, 0:nk * 512], reads=sf_res, writes=sb_res)
            else:
                S.add("dve", "tensor_copy", out=sbb[:, 0:nk * 512], in_=sf_ap[:, 0:nk * 512], reads=sf_res, writes=sb_res)
            S.add("sp", "dma_start", out=wsc[b, :, 0:nk * 512], in_=sbb[:, 0:nk * 512],
                  reads=sb_res, writes=[("wsc", b)], dma_sem=f"D_ps{pi % NST}")

        for pi in range(min(NST - 1, len(ub))):
            pl_load(pi)
        for pi in range(len(ub)):
            if pi + NST - 1 < len(ub):
                pl_load(pi + NST - 1)
            pl_cast_store(pi)

        seq = []
        for t in range(ntiles):
            for b in range(NBLK):
                if b in used_blocks:
                    seq.append(b)
        wstate = {"next_load": 0, "next_use": 0}

        def wnext(hold=0):
            i = wstate["next_use"]
            wstate["next_use"] += 1
            while wstate["next_load"] < len(seq) and wstate["next_load"] <= i + NSLOT - 1 - hold:
                j = wstate["next_load"]
                b = seq[j]
                nk = WB[b][3]
                s = j % NSLOT
                S.add("sp", "dma_start", out=ws[s][:, 0:nk * 512], in_=wsc[b, :, 0:nk * 512],
                      reads=[("wsc", b)], writes=[("ws", s)], dma_sem=f"D_w{s}")
                wstate["next_load"] += 1
            s = i % NSLOT
            return ws[s], ("ws", s), seq[i]

        def rms_stats(tts):
            x_t = cur["x"]
            a, b_ = tts[0], tts[-1] + 1
            for tt in tts:
                S.add("act", "activation", out=osq[:, :], in_=x_t[:, tt * 1024:(tt + 1) * 1024],
                      func=AF.Square, accum_out=ss[:, tt:tt + 1],
                      reads=xres(tt), writes=[("osq", 0), ("osq", 1), ("ss", tt)])
            S.add("act", "activation", out=rs[:, a:b_], in_=ss[:, a:b_], func=AF.Ln,
                  scale=1.0 / D, bias=cst[:, 0:1], reads=[("ss", tt) for tt in tts] + ["cst"],
                  writes=[("rs", tt) for tt in tts])
            S.add("act", "activation", out=rs[:, a:b_], in_=rs[:, a:b_], func=AF.Exp, scale=-0.5,
                  reads=[("rs", tt) for tt in tts], writes=[("rs", tt) for tt in tts])

        def rms_stats_all():
            rms_stats([0, 1, 2, 3])

        def norm_to_hT_part(tts):
            x_t = cur["x"]
            rms_stats(tts)
            for p0 in range(0, len(tts), 2):
                pair = tts[p0:p0 + 2]
                for tt in pair:
                    hp = hpre[:, (tt % 2) * 1024:(tt % 2 + 1) * 1024]
                    if tt % 2 == 0:
                        S.add("act", "activation", out=hp, in_=x_t[:, tt * 1024:(tt + 1) * 1024], func=AF.Copy,
                              scale=rs[:, tt:tt + 1], reads=xres(tt) + [("rs", tt)], writes=[("hpre", tt % 2)])
                    else:
                        S.add("dve", "tensor_scalar", out=hp, in0=x_t[:, tt * 1024:(tt + 1) * 1024],
                              scalar1=rs[:, tt:tt + 1], scalar2=None, op0=ALU.mult,
                              reads=xres(tt) + [("rs", tt)], writes=[("hpre", tt % 2)])
                for tt in pair:
                    hp = hpre[:, (tt % 2) * 1024:(tt % 2 + 1) * 1024]
                    k = bank()
                    pb = ps[k][:, :].bitcast(BF16)
                    for kc in range(8):
                        S.add("pe", "transpose", out=pb[:, kc * 128:(kc + 1) * 128], in_=hp[:, kc * 128:(kc + 1) * 128],
                              identity=ident[:, :], reads=[("hpre", tt % 2), "ident"], writes=[("ps", k)])
                    dst = hT[:, :].rearrange("p (kc t) -> p kc t", t=512)[:, :, tt * 128:(tt + 1) * 128]
                    srcv = pb.rearrange("p (kc t) -> p kc t", t=128)
                    if tt % 2 == 0:
                        S.add("dve", "tensor_copy", out=dst, in_=srcv, reads=[("ps", k)], writes=[("hT", tt)])
                    else:
                        S.add("act", "copy", out=dst, in_=srcv, reads=[("ps", k)], writes=[("hT", tt)])

        def norm_to_hT(gi):
            norm_to_hT_part([0, 1, 2, 3])

        HT_ALL = [("hT", tt) for tt in range(4)]

        def proj_fm(wt, wres, j):
            k = bank()
            for kc in range(8):
                S.add("pe", "matmul", ps[k][:, :], lhsT=wt[:, kc * 512 + j * 128:kc * 512 + (j + 1) * 128],
                      rhs=hT[:, kc * 512:(kc + 1) * 512], start=(kc == 0), stop=(kc == 7),
                      reads=[wres] + HT_ALL, writes=[("ps", k)])
            return k

        def proj_tm(wt, wres, tt, src, src_res, nk=8, koff=0, k=None, first=True, last=True):
            if k is None:
                k = bank()
            for kc in range(nk):
                S.add("pe", "matmul", ps[k][:, :],
                      lhsT=src[:, (koff + kc) * 512 + tt * 128:(koff + kc) * 512 + (tt + 1) * 128],
                      rhs=wt[:, kc * 512:(kc + 1) * 512], start=(first and kc == 0), stop=(last and kc == nk - 1),
                      reads=[wres] + src_res, writes=[("ps", k)])
            return k

        def decay_tail(h, bsrc, bres, sign, dk_scale, eq_col, ek_dst, ek_res):
            eqk = cur["eqk"]
            S.add("act", "activation", out=eqk[:, eq_col:eq_col + 512], in_=bsrc, func=AF.Exp,
                  scale=sign * dk_scale, bias=cst[:, 2:3], reads=[bres, "cst"], writes=[eres(eq_col // 512)])
            S.add("act", "activation", out=ek_dst, in_=bsrc, func=AF.Exp, scale=-sign * dk_scale,
                  reads=[bres], writes=[ek_res])
            S.add("act", "activation", out=eblast[:, h * 8:(h + 1) * 8], in_=bsrc[:, 63:512:64], func=AF.Exp,
                  scale=sign * dk_scale, reads=[bres], writes=[("ebl", h)])

        def kd_scale(h):
            S.add("pool", "tensor_tensor", out=kdT_all[:, h * 512:(h + 1) * 512].rearrange("p (c t) -> p c t", t=64),
                  in0=kbog[:, h * 512:(h + 1) * 512].rearrange("p (c t) -> p c t", t=64),
                  in1=eblast[:, h * 8:(h + 1) * 8].unsqueeze(2).to_broadcast([128, 8, 64]), op=ALU.mult,
                  reads=[("kbog", h), ("ebl", h)], writes=[("act", h)])

        def kd_transpose(h):
            k = bank()
            pb = ps[k][:, :].bitcast(BF16)
            for tt in range(4):
                S.add("pe", "transpose", out=pb[:, tt * 128:(tt + 1) * 128],
                      in_=kdT_all[:, h * 512 + tt * 128:h * 512 + (tt + 1) * 128],
                      identity=ident[:, :], reads=[("act", h), "ident"], writes=[("ps", k)])
            S.add("dve", "tensor_copy",
                  out=kd_tok[:, :].rearrange("p (tt f) -> p tt f", f=1024)[:, :, h * 128:(h + 1) * 128],
                  in_=pb[:, 0:512].rearrange("p (tt f) -> p tt f", f=128),
                  reads=[("ps", k)], writes=[("kd_tok", h)])

        def attention_AT(H):
            AT_sb = cur["AT"]
            for h in range(H):
                k = bank()
                for tt in range(4):
                    S.add("pe", "matmul", ps[k][:, tt * 128:(tt + 1) * 128],
                          lhsT=kbog[:, h * 512 + tt * 128:h * 512 + (tt + 1) * 128],
                          rhs=qb[:, h * 512 + tt * 128:h * 512 + (tt + 1) * 128], start=True, stop=True,
                          reads=[("kbog", h), ("qb", h)], writes=[("ps", k)])
                S.add("dve", "tensor_tensor", out=AT_sb[:, h * 512:(h + 1) * 512], in0=ps[k][:, :], in1=mask4[:, :],
                      op=ALU.mult, reads=[("ps", k), "mask4"], writes=[eres(h // 2)])

        def attention_rec_init(tile_in_seq, S_f):
            if tile_in_seq == 0:
                S.add("dve", "memset", S_f[:, :], 0.0, writes=["S_f"])
            S.add("act", "copy", out=Sbf[0][:, :], in_=S_f[:, :], reads=["S_f"], writes=[("Sbf", 0)])

        def attention_rec_chunk(H, dv, S_f, c):
            hpb = 512 // dv
            tt, po = c // 2, (c % 2) * 64
            kA, kB = bank(), bank()
            for h in range(H):
                k = kA if h < hpb else kB
                col = (h % hpb) * dv
                S.add("pe", "matmul", ps[k][:, col:col + dv],
                      lhsT=kd_tok[po:po + 64, tt * 1024 + h * 128:tt * 1024 + (h + 1) * 128],
                      rhs=v_tok[po:po + 64, tt * 1024 + h * dv:tt * 1024 + (h + 1) * dv], start=True, stop=True,
                      reads=[("kd_tok", h), ("v_tok", tt)], writes=[("ps", k)])
            for h in range(H):
                k = kA if h < hpb else kB
                col = (h % hpb) * dv
                S.add("dve", "scalar_tensor_tensor", out=S_f[:, h * dv:(h + 1) * dv], in0=S_f[:, h * dv:(h + 1) * dv],
                      scalar=eblast[:, h * 8 + c:h * 8 + c + 1], in1=ps[k][:, col:col + dv],
                      op0=ALU.mult, op1=ALU.add,
                      reads=["S_f", ("ebl", h), ("ps", k)], writes=["S_f"])
            S.add("act", "copy", out=Sbf[c + 1][:, :], in_=S_f[:, :], reads=["S_f"], writes=[("Sbf", c + 1)])

        def attention_out(H, dv, ogn, ogn_res, groups, tok_rstd=False):
            nd = dv // 128
            AT_sb = cur["AT"]
            for g in groups:
                kS = bank(pin=True)
                for gi_, j in enumerate(g):
                    h = (j * 128) // dv
                    k = bank()
                    for tt in range(4):
                        S.add("pe", "matmul", ps[k][:, tt * 128:(tt + 1) * 128],
                              lhsT=v_tok[:, tt * 1024 + j * 128:tt * 1024 + (j + 1) * 128],
                              rhs=AT_sb[:, h * 512 + tt * 128:h * 512 + (tt + 1) * 128], start=True, stop=False,
                              reads=[("v_tok", tt), eres(h // 2)], writes=[("ps", k)])
                        for cc in range(2):
                            c = 2 * tt + cc
                            S.add("pe", "matmul", ps[k][:, c * 64:(c + 1) * 64],
                                  lhsT=Sbf[c][:, j * 128:(j + 1) * 128],
                                  rhs=qb[:, h * 512 + c * 64:h * 512 + (c + 1) * 64], start=False, stop=(cc == 1),
                                  reads=[("Sbf", c), ("qb", h)], writes=[("ps", k)])
                    if gi_ > 0:
                        jp = g[gi_ - 1]
                        S.add("pe", "matmul", ps[kS][:, :], lhsT=ones_bf[:, :], rhs=osq[:, (jp % 2) * 512:(jp % 2 + 1) * 512],
                              start=(gi_ - 1 == 0), stop=False,
                              reads=["ones_bf", ("osq", jp % 2)], writes=[("ps", kS)])
                    oq = osq[:, (j % 2) * 512:(j % 2 + 1) * 512]
                    S.add("act", "activation", out=oq, in_=ps[k][:, :], func=AF.Square,
                          reads=[("ps", k)], writes=[("osq", j % 2)])
                    oi = (j % nd) if nd > 1 else j
                    if tok_rstd:
                        S.add("dve", "scalar_tensor_tensor", out=kbog[:, j * 512:(j + 1) * 512], in0=ps[k][:, :],
                              scalar=ogn[:, oi:oi + 1],
                              in1=gate[:, j * 512:(j + 1) * 512], op0=ALU.mult, op1=ALU.mult,
                              reads=[("ps", k), ogn_res, ("gate", j), ("osq", j % 2)],
                              writes=[("kbog", j)])
                    else:
                        S.add("dve", "scalar_tensor_tensor", out=t1[:, j * 512:(j + 1) * 512], in0=ps[k][:, :],
                              scalar=ogn[:, oi:oi + 1],
                              in1=gate[:, j * 512:(j + 1) * 512], op0=ALU.mult, op1=ALU.mult,
                              reads=[("ps", k), ogn_res, ("gate", j), ("osq", j % 2)],
                              writes=[("act", 2 * j), ("act", 2 * j + 1)])
                    if gi_ == len(g) - 1:
                        S.add("pe", "matmul", ps[kS][:, :], lhsT=ones_bf[:, :], rhs=oq,
                              start=(gi_ == 0), stop=True,
                              reads=["ones_bf", ("osq", j % 2)], writes=[("ps", kS)])
                S.add("act", "activation", out=rstd_t[:, :], in_=ps[kS][:, :], func=AF.Ln,
                      scale=1.0 / (128 * len(g)), bias=cst[:, 0:1], reads=[("ps", kS), "cst"], writes=["rstd_t"])
                S.add("act", "activation", out=rstd_t[:, :], in_=rstd_t[:, :], func=AF.Exp, scale=-0.5,
                      reads=["rstd_t"], writes=["rstd_t"])
                pinned.discard(kS)
                if tok_rstd:
                    continue
                for ji, j in enumerate(g):
                    eng = "dve" if ji % 2 == 0 else "pool"
                    S.add(eng, "tensor_tensor", out=kbog[:, j * 512:(j + 1) * 512], in0=t1[:, j * 512:(j + 1) * 512],
                          in1=rstd_t[:, :], op=ALU.mult,
                          reads=[("act", 2 * j), ("act", 2 * j + 1), "rstd_t"], writes=[("kbog", j)])

        def residual_add(k, tt, ob):
            x_t = cur["x"]
            S.add("dve", "tensor_tensor", out=x_t[:, tt * 1024 + ob * 512:tt * 1024 + (ob + 1) * 512],
                  in0=x_t[:, tt * 1024 + ob * 512:tt * 1024 + (ob + 1) * 512], in1=ps[k][:, :], op=ALU.add,
                  reads=xres(tt) + [("ps", k)], writes=xres(tt))

        def out_proj_residual(tok_rstd=False):
            OG = [("kbog", j) for j in range(8)]
            x_t = cur["x"]
            w = [wnext(), wnext(hold=1)]

            def mm(tts):
                out = []
                for ob in range(2):
                    for tt in tts:
                        out.append((proj_tm(w[ob][0], w[ob][1], tt, kbog, OG), tt, ob))
                return out

            def adds(lst):
                for k, tt, ob in lst:
                    if tok_rstd:
                        xs = x_t[:, tt * 1024 + ob * 512:tt * 1024 + (ob + 1) * 512]
                        S.add("dve", "scalar_tensor_tensor", out=xs, in0=ps[k][:, :], scalar=rsd_tok[:, tt:tt + 1],
                              in1=xs, op0=ALU.mult, op1=ALU.add,
                              reads=xres(tt) + [("ps", k), "rsd_tok"], writes=xres(tt))
                    else:
                        residual_add(k, tt, ob)

            h0 = mm([0, 1])
            if tok_rstd:
                kR = bank()
                for t2 in range(4):
                    S.add("pe", "matmul", ps[kR][:, t2:t2 + 1], lhsT=rstd_t[0:1, t2 * 128:(t2 + 1) * 128],
                          rhs=cst[0:1, 1:2], start=True, stop=True,
                          reads=["rstd_t", "cst"], writes=[("ps", kR)])
                S.add("act", "copy", out=rsd_tok[:, 0:4], in_=ps[kR][:, 0:4], reads=[("ps", kR)], writes=["rsd_tok"])
            adds(h0)
            h1 = mm([2, 3])
            norm_to_hT_part([0, 1])
            adds(h1)
            norm_to_hT_part([2, 3])

        def ffn_in(layer, do_norm=True):
            if do_norm:
                norm_to_hT(2 + layer)
            for j in range(11):
                wt, wres, _ = wnext()
                for ch in range(2):
                    kg = proj_fm(wt, wres, ch)
                    ku = proj_fm(wt, wres, 2 + ch)
                    sgt = sg[:, (ch % 2) * 512:(ch % 2 + 1) * 512]
                    S.add("act", "activation", out=sgt, in_=ps[kg][:, :], func=AF.Silu,
                          reads=[("ps", kg)], writes=[("sg", ch % 2)])
                    cidx = 2 * j + ch
                    S.add("dve", "tensor_tensor", out=act[:, cidx * 512:(cidx + 1) * 512], in0=ps[ku][:, :], in1=sgt,
                          op=ALU.mult, reads=[("ps", ku), ("sg", ch % 2)], writes=[("act", cidx)])

        def ffn_out(layer, between=None):
            ACT_ALL = [("act", c) for c in range(22)]
            for ob in range(2):
                if ob == 1 and between is not None:
                    between()
                ks = [bank() for _ in range(4)]
                for kr in range(3):
                    wt, wres, b = wnext()
                    nk = WB[b][3]
                    for tt in range(4):
                        proj_tm(wt, wres, tt, act, ACT_ALL, nk=nk, koff=8 * kr, k=ks[tt], first=(kr == 0), last=(kr == 2))
                for tt in range(4):
                    residual_add(ks[tt], tt, ob)

        def v_proj(vbs=(0, 1)):
            for vb in vbs:
                wt, wres, _ = wnext()
                for tt in range(4):
                    k = proj_tm(wt, wres, tt, hT, HT_ALL)
                    dst = v_tok[:, tt * 1024 + vb * 512:tt * 1024 + (vb + 1) * 512]
                    S.add("dve", "tensor_copy", out=dst, in_=ps[k][:, :], reads=[("ps", k)], writes=[("v_tok", tt)])

        def gate_groups():
            out = []
            for rb in range(2):
                holder = {}
                for jj in range(4):
                    def g(rb=rb, jj=jj, holder=holder):
                        if jj == 0:
                            holder["w"] = wnext()
                        wt, wres, _ = holder["w"]
                        k = proj_fm(wt, wres, jj)
                        j = rb * 4 + jj
                        S.add("act", "activation", out=gate[:, j * 512:(j + 1) * 512], in_=ps[k][:, :], func=AF.Silu,
                              reads=[("ps", k)], writes=[("gate", j)])
                    out.append(g)
            return out

        def gla_q_groups():
            eqk = cur["eqk"]
            out = []
            holder = {}
            for h in range(4):
                def g(h=h, holder=holder):
                    if h == 0:
                        holder["w"] = wnext()
                    wt, wres, _ = holder["w"]
                    k = proj_fm(wt, wres, h)
                    S.add("dve", "tensor_tensor", out=qb[:, h * 512:(h + 1) * 512], in0=ps[k][:, :],
                          in1=eqk[:, h * 512:(h + 1) * 512], op=ALU.mult,
                          reads=[("ps", k), eres(h)], writes=[("qb", h)])
                out.append(g)
            return out

        def hgrn_q_groups():
            eqk = cur["eqk"]
            out = []
            for qblk in range(2):
                holder = {}
                for hh in range(4):
                    def g(qblk=qblk, hh=hh, holder=holder):
                        if hh == 0:
                            holder["w"] = wnext()
                        wt, wres, _ = holder["w"]
                        h = qblk * 4 + hh
                        k = proj_fm(wt, wres, hh)
                        ta = gL[:, (h % 2) * 512:(h % 2 + 1) * 512]
                        S.add("act", "activation", out=ta, in_=ps[k][:, :], func=AF.Silu,
                              reads=[("ps", k)], writes=[("gL", h % 2)])
                        S.add("dve", "tensor_tensor", out=qb[:, h * 512:(h + 1) * 512], in0=ta,
                              in1=eqk[:, h * 512:(h + 1) * 512], op=ALU.mult,
                              reads=[("gL", h % 2), eres(h)], writes=[("qb", h)])
                    out.append(g)
            return out

        def gla_layer(tile_in_seq, after_attention, do_norm=True):
            eqk = cur["eqk"]
            if do_norm:
                norm_to_hT(0)
            k = bank()
            for kc in range(8):
                S.add("pe", "matmul", ps[k][0:16, :], lhsT=wgd[:, kc * 16:(kc + 1) * 16], rhs=hT[:, kc * 512:(kc + 1) * 512],
                      start=(kc == 0), stop=(kc == 7), reads=["wgd"] + HT_ALL, writes=[("ps", k)])
            S.add("act", "copy", out=gd_sb[0:16, :], in_=ps[k][0:16, :], reads=[("ps", k)], writes=["gd_sb"])

            def stage1(h):
                k = bank()
                S.add("pe", "matmul", ps[k][:, :], lhsT=wgu[0:16, h * 128:(h + 1) * 128], rhs=gd_sb[0:16, :],
                      start=True, stop=True, reads=["wgu", "gd_sb"], writes=[("ps", k)])
                ta = tmpA[:, (h % 3) * 512:(h % 3 + 1) * 512]
                S.add("act", "activation", out=ta, in_=ps[k][:, :], func=AF.Exp, scale=-1.0, bias=negbg[:, h:h + 1],
                      reads=[("ps", k), "negbg"], writes=[("tmpA", h % 3)])
                gl = gL[:, (h % 2) * 512:(h % 2 + 1) * 512]
                S.add("act", "activation", out=gl, in_=ta, func=AF.Ln, scale=1.0, bias=cst[:, 1:2],
                      reads=[("tmpA", h % 3), "cst"], writes=[("gL", h % 2)])
                bc = bcr[:, (h % 3) * 512:(h % 3 + 1) * 512]
                S.add("dve", "tensor_tensor_scan", out=bc, data0=maskc[:, :], data1=gl, initial=0.0,
                      op0=ALU.mult, op1=ALU.add, reads=["maskc", ("gL", h % 2)], writes=[("bcr", h % 3)])

            def stage2(h):
                bc = bcr[:, (h % 3) * 512:(h % 3 + 1) * 512]
                decay_tail(h, bc, ("bcr", h % 3), -1.0, 1.0 / 16.0, h * 512,
                           eqk[:, 2048 + h * 512:2048 + (h + 1) * 512], eres(4 + h))

            v_proj((0,))
            for h in range(4):
                stage1(h)
                if h >= 1:
                    stage2(h - 1)
            stage2(3)
            v_proj((1,))
            wt, wres, _ = wnext()
            for h in range(4):
                k = proj_fm(wt, wres, h)
                S.add("dve", "tensor_tensor", out=kbog[:, h * 512:(h + 1) * 512], in0=ps[k][:, :],
                      in1=eqk[:, 2048 + h * 512:2048 + (h + 1) * 512], op=ALU.mult,
                      reads=[("ps", k), eres(4 + h)], writes=[("kbog", h)])
                kd_scale(h)
            groups = gate_groups() + gla_q_groups()
            gi = 0
            for _ in range(2):
                groups[gi]()
                gi += 1
            for h in range(4):
                kd_transpose(h)
            attention_rec_init(tile_in_seq, S_fs[0])
            for c in range(NCH):
                attention_rec_chunk(4, 256, S_fs[0], c)
                n = 2 if c < 2 else 1
                for _ in range(n):
                    groups[gi]()
                    gi += 1
            assert gi == len(groups)
            attention_AT(4)
            attention_out(4, 256, ognorm0, "ognorm0", [[0, 1], [2, 3], [4, 5], [6, 7]])
            after_attention()
            out_proj_residual()

        def hgrn_layer(tile_in_seq, after_attention, do_norm=True):
            eqk = cur["eqk"]
            if do_norm:
                norm_to_hT(1)
            fw = {}

            def stage1(h):
                if h % 4 == 0:
                    fw["w"] = wnext()
                wt, wres, _ = fw["w"]
                k = proj_fm(wt, wres, h % 4)
                ta = tmpA[:, (h % 3) * 512:(h % 3 + 1) * 512]
                gl = gL[:, (h % 2) * 512:(h % 2 + 1) * 512]
                l1 = sg[:, (h % 2) * 512:(h % 2 + 1) * 512]
                S.add("act", "activation", out=ta, in_=ps[k][:, :], func=AF.Exp, scale=-1.0,
                      reads=[("ps", k)], writes=[("tmpA", h % 3)])
                S.add("act", "activation", out=l1, in_=ta, func=AF.Ln, scale=1.0, bias=cst[:, 1:2],
                      reads=[("tmpA", h % 3), "cst"], writes=[("sg", h % 2)])
                S.add("act", "activation", out=gl, in_=ta, func=AF.Ln, scale=lb_t[:, h:h + 1], bias=cst[:, 1:2],
                      reads=[("tmpA", h % 3), "cst", "lb"], writes=[("gL", h % 2)])
                S.add("dve", "tensor_tensor", out=gl, in0=gl, in1=l1, op=ALU.subtract,
                      reads=[("gL", h % 2), ("sg", h % 2)], writes=[("gL", h % 2)])
                bc = bcr[:, (h % 3) * 512:(h % 3 + 1) * 512]
                S.add("dve", "tensor_tensor_scan", out=bc, data0=maskc[:, :], data1=gl, initial=0.0,
                      op0=ALU.mult, op1=ALU.add, reads=["maskc", ("gL", h % 2)], writes=[("bcr", h % 3)])
                S.add("act", "activation", out=ta, in_=l1, func=AF.Exp, scale=-1.0,
                      reads=[("sg", h % 2)], writes=[("tmpA", h % 3)])
                S.add("dve", "tensor_scalar", out=ta, in0=ta, scalar1=noml[:, h:h + 1], scalar2=oml[:, h:h + 1],
                      op0=ALU.mult, op1=ALU.add, reads=[("tmpA", h % 3), "noml", "oml"], writes=[("tmpA", h % 3)])

            def stage2(h):
                ta = tmpA[:, (h % 3) * 512:(h % 3 + 1) * 512]
                bc = bcr[:, (h % 3) * 512:(h % 3 + 1) * 512]
                ekt = sg[:, (h % 2) * 512:(h % 2 + 1) * 512]
                decay_tail(h, bc, ("bcr", h % 3), 1.0, 1.0, h * 512, ekt, ("sg", h % 2))
                S.add("dve", "tensor_tensor", out=kbog[:, h * 512:(h + 1) * 512], in0=ta, in1=ekt, op=ALU.mult,
                      reads=[("tmpA", h % 3), ("sg", h % 2)], writes=[("kbog", h)])
                kd_scale(h)

            for h in range(8):
                stage1(h)
                if h >= 1:
                    stage2(h - 1)
            stage2(7)
            v_proj()
            groups = hgrn_q_groups() + gate_groups()
            gi = 0
            for _ in range(4):
                groups[gi]()
                gi += 1
            for h in range(8):
                kd_transpose(h)
            attention_rec_init(tile_in_seq, S_fs[1])
            for c in range(NCH):
                attention_rec_chunk(8, 128, S_fs[1], c)
                n = 2 if c < 4 else 1
                for _ in range(n):
                    groups[gi]()
                    gi += 1
            assert gi == len(groups)
            attention_AT(8)
            attention_out(8, 128, ognorm1, "ognorm1", [list(range(8))], tok_rstd=True)
            after_attention()
            out_proj_residual(tok_rstd=True)

        def load_x(t):
            p = t % 2
            S.add("pool", "dma_start", out=XB[p][:, :].rearrange("p (tt d) -> p tt d", d=1024),
                  in_=x_in[t * T:(t + 1) * T, :].rearrange("(tt p) d -> p tt d", p=128),
                  writes=xall(p), dma_sem=f"D_x{p}")

        load_x(0)
        last_layer = max(layers)
        first_layer = min(layers)
        set_tile_bufs(0)
        norm_to_hT(first_layer)
        for t in range(ntiles):
            tis = t % tiles_per_seq
            set_tile_bufs(t)

            def prefetch_next(t=t):
                if t + 1 < ntiles:
                    load_x(t + 1)

            def nop():
                pass

            def pre_norm_next(t=t):
                if t + 1 < ntiles:
                    set_tile_bufs(t + 1)
                    norm_to_hT(first_layer)
                    set_tile_bufs(t)

            if 0 in layers:
                gla_layer(tis, prefetch_next if last_layer == 0 else nop, do_norm=False)
                ffn_in(0, do_norm=False)
                ffn_out(0, between=(pre_norm_next if last_layer == 0 else None))
            if 1 in layers:
                hgrn_layer(tis, prefetch_next if last_layer == 1 else nop, do_norm=(first_layer != 1))
                ffn_in(1, do_norm=False)
                ffn_out(1, between=pre_norm_next)
            x_t = cur["x"]
            if final_norm:
                rms_stats_all()
                for tt in range(4):
                    S.add("dve", "scalar_tensor_tensor", out=x_t[:, tt * 1024:(tt + 1) * 1024],
                          in0=x_t[:, tt * 1024:(tt + 1) * 1024], scalar=rs[:, tt:tt + 1], in1=gfin[:, :],
                          op0=ALU.mult, op1=ALU.mult, reads=xres(tt) + [("rs", tt), "gfin"], writes=xres(tt))
            S.add("pool", "dma_start", out=y_out[t * T:(t + 1) * T, :].rearrange("(tt p) d -> p tt d", p=128),
                  in_=x_t[:, :].rearrange("p (tt d) -> p tt d", d=1024),
                  reads=xall(), writes=[("y", t)], dma_sem=f"D_y{t % 2}")
        S.add("pool", None, reads=[("y", t) for t in range(ntiles)])
        S.emit(nc, es)
    return nc


_PARAM_NAMES = ["mixer_norm", "ffn_norm", "gla_w_in", "gla_w_gate_up", "gla_b_gate", "gla_head_norm",
                "gla_w_out", "hgrn_w_in", "hgrn_lower_bounds", "hgrn_out_norm", "hgrn_w_out",
                "ffn_w_in", "ffn_w_out", "final_norm"]


def run_module(inputs, layers=(0, 1), final_norm=True):
    x = np.ascontiguousarray(np.asarray(inputs["x"], dtype=np.float32))
    B, SEQ, _ = x.shape
    assert B % NCORES == 0 and SEQ % T == 0
    bpc = B // NCORES
    tiles_per_seq = SEQ // T
    ntiles = bpc * tiles_per_seq
    nc = build_program(ntiles, tiles_per_seq, layers=layers, final_norm=final_norm)
    params = {k: np.ascontiguousarray(np.asarray(inputs[k], dtype=np.float32)) for k in _PARAM_NAMES}
    in_maps = []
    for c in range(NCORES):
        m = {"x": x[c * bpc:(c + 1) * bpc].reshape(bpc * SEQ, D)}
        m.update(params)
        in_maps.append(m)
    res = run_bass_kernel_spmd(nc, in_maps, core_ids=list(range(NCORES)))
    out = np.concatenate([np.asarray(r["y"]).reshape(bpc, SEQ, D) for r in res.results], axis=0)
    return out.astype(np.float32)


def kernel(**inputs):
    return run_module(inputs)
```
